# Optimizing a Trainium2 kernel written in Bass

```python
import jax, jax.numpy as jnp
from jax import lax
import numpy as np

D_MODEL = 2048
BATCH = 2
SEQ = 8192
DEPTH = 2

GRID_W = 64
CTX_LEN = 256
N_MIXERS = 2
N_HEADS = 16
N_KV_HEADS = 4
HEAD_DIM = D_MODEL // N_HEADS
Q_PER_KV = N_HEADS // N_KV_HEADS
KV_DIM = N_KV_HEADS * HEAD_DIM
QKV_DIM = D_MODEL + 2 * KV_DIM
ATTN_SCALE = HEAD_DIM ** -0.5
ROPE_THETA = 10000.0
ROPE_PAIRS = HEAD_DIM // 4
Q_BLOCK = 128
POOL_WINDOWS = (2, 4, 8, 16)
N_POOL_GROUPS = len(POOL_WINDOWS)
POOL_GROUP_DIM = D_MODEL // N_POOL_GROUPS
N_EXPERTS = 32
N_EXPERT_GROUPS = 8
EXPERTS_PER_GROUP = N_EXPERTS // N_EXPERT_GROUPS
TOP_K = 2
D_EXPERT = D_MODEL // 2
MOE_BLOCK = 256
N_ATTN_LAYERS = (DEPTH + 1) // 2
N_POOL_LAYERS = DEPTH // 2
DEEPNORM_ALPHA = (2 * DEPTH) ** 0.25
DEEPNORM_BETA = (8 * DEPTH) ** -0.25
LN_EPS = 1e-6
RMS_EPS = 1e-6

kernel_name = 'hybrid_gqa_pool_grouped_moe_deepnorm_dit'


def layer_norm(x, g, b):
    xf = x.astype(jnp.float32)
    mu = jnp.mean(xf, axis=-1, keepdims=True)
    var = jnp.mean(jnp.square(xf - mu), axis=-1, keepdims=True)
    y = (xf - mu) * lax.rsqrt(var + LN_EPS) * g.astype(jnp.float32) + b.astype(jnp.float32)
    return y.astype(x.dtype)


def rms_norm(x, g):
    xf = x.astype(jnp.float32)
    y = xf * lax.rsqrt(jnp.mean(jnp.square(xf), axis=-1, keepdims=True) + RMS_EPS) * g.astype(jnp.float32)
    return y.astype(x.dtype)


def modulate(x, shift, scale):
    return x * (1 + scale) + shift


def axial_rope_tables(n_rows, dtype):
    t = jnp.arange(n_rows * GRID_W)
    row = (t // GRID_W).astype(jnp.float32)
    col = (t % GRID_W).astype(jnp.float32)
    inv_freq = ROPE_THETA ** (-jnp.arange(ROPE_PAIRS, dtype=jnp.float32) / ROPE_PAIRS)
    ang_r = row[:, None] * inv_freq
    ang_c = col[:, None] * inv_freq
    ang = jnp.concatenate([ang_r, ang_r, ang_c, ang_c], axis=-1)
    return jnp.cos(ang).astype(dtype), jnp.sin(ang).astype(dtype)


def apply_axial_rope(x, cos, sin):
    xr = x.reshape(x.shape[:-1] + (2, 2, ROPE_PAIRS))
    rot = jnp.stack([-xr[..., 1, :], xr[..., 0, :]], axis=-2).reshape(x.shape)
    return x * cos[:, None, :] + rot * sin[:, None, :]


def project_qkv(u, w_qkv, q_gain, k_gain):
    B, L, _ = u.shape
    qkv = u @ w_qkv
    q = qkv[..., :D_MODEL].reshape(B, L, N_HEADS, HEAD_DIM)
    k = qkv[..., D_MODEL:D_MODEL + KV_DIM].reshape(B, L, N_KV_HEADS, HEAD_DIM)
    v = qkv[..., D_MODEL + KV_DIM:].reshape(B, L, N_KV_HEADS, HEAD_DIM)
    return rms_norm(q, q_gain), rms_norm(k, k_gain), v


def project_kv(u, w_kv, k_gain):
    B, L, _ = u.shape
    kv = u @ w_kv
    k = kv[..., :KV_DIM].reshape(B, L, N_KV_HEADS, HEAD_DIM)
    v = kv[..., KV_DIM:].reshape(B, L, N_KV_HEADS, HEAD_DIM)
    return rms_norm(k, k_gain), v


def gqa_attend(q, k, v):
    s = jnp.einsum('bqkgd,bskd->bkgqs', q, k, preferred_element_type=jnp.float32) * ATTN_SCALE
    p = jax.nn.softmax(s, axis=-1).astype(v.dtype)
    return jnp.einsum('bkgqs,bskd->bqkgd', p, v)


def attention_mixer(u, uc, rope_cos, rope_sin, w_qkv, q_gain, k_gain, w_o, need_ctx):
    B, L, _ = u.shape
    C = uc.shape[1]
    q, k, v = project_qkv(u, w_qkv, q_gain, k_gain)
    q = apply_axial_rope(q, rope_cos, rope_sin)
    k = apply_axial_rope(k, rope_cos, rope_sin)
    if need_ctx:
        qc, kc, vc = project_qkv(uc, w_qkv, q_gain, k_gain)
    else:
        kc, vc = project_kv(uc, w_qkv[:, D_MODEL:], k_gain)
    k_all = jnp.concatenate([kc, k], axis=1)
    v_all = jnp.concatenate([vc, v], axis=1)
    n_blk = L // Q_BLOCK
    q_blocks = q.reshape(B, n_blk, Q_BLOCK, N_KV_HEADS, Q_PER_KV, HEAD_DIM).swapaxes(0, 1)
    o = lax.map(lambda qb: gqa_attend(qb, k_all, v_all), q_blocks)
    y = o.swapaxes(0, 1).reshape(B, L, D_MODEL) @ w_o
    if not need_ctx:
        return y, None
    oc = gqa_attend(qc.reshape(B, C, N_KV_HEADS, Q_PER_KV, HEAD_DIM), kc, vc)
    return y, oc.reshape(B, C, D_MODEL) @ w_o


def pool_mixer(u, w_pool, pool_scale):
    B, L, D = u.shape
    uf = u.astype(jnp.float32)
    cs = jnp.concatenate([jnp.zeros((B, 1, D), jnp.float32), jnp.cumsum(uf, axis=1)], axis=1)
    t = jnp.arange(L)
    outs = []
    for g, w in enumerate(POOL_WINDOWS):
        sl = slice(g * POOL_GROUP_DIM, (g + 1) * POOL_GROUP_DIM)
        lo = jnp.clip(t - w // 2, 0, L)
        hi = jnp.clip(t + w // 2, 0, L)
        cnt = (hi - lo).astype(jnp.float32)[:, None]
        csg = cs[..., sl]
        mean = (csg[:, hi] - csg[:, lo]) / cnt
        d = (mean - uf[..., sl]).astype(u.dtype)
        outs.append(d @ w_pool[g])
    return jnp.concatenate(outs, axis=-1) * pool_scale


def moe_ffn(h, w_router, router_bias, w_gate, w_up, w_down):
    T = h.shape[0]
    n_assign = T * TOP_K
    logits = jnp.einsum('td,de->te', h, w_router, preferred_element_type=jnp.float32)
    aff = jax.nn.sigmoid(logits)
    sel = (aff + router_bias.astype(jnp.float32)).reshape(T, N_EXPERT_GROUPS, EXPERTS_PER_GROUP)
    group_score = jnp.sum(lax.top_k(sel, 2)[0], axis=-1)
    g_idx = jnp.argmax(group_score, axis=-1)
    in_group = sel[jnp.arange(T), g_idx]
    _, local = lax.top_k(in_group, TOP_K)
    e_idx = g_idx[:, None] * EXPERTS_PER_GROUP + local
    gate = jnp.take_along_axis(aff, e_idx, axis=1)
    gate = gate / jnp.sum(gate, axis=-1, keepdims=True)
    flat_e = e_idx.reshape(-1).astype(jnp.int32)
    flat_tok = jnp.repeat(jnp.arange(T, dtype=jnp.int32), TOP_K)
    flat_gate = gate.reshape(-1)
    order = jnp.argsort(flat_e, stable=True)
    se = flat_e[order]
    counts = jnp.bincount(flat_e, length=N_EXPERTS)
    padded = (counts + MOE_BLOCK - 1) // MOE_BLOCK * MOE_BLOCK
    starts = jnp.cumsum(counts) - counts
    p_ends = jnp.cumsum(padded)
    p_starts = p_ends - padded
    dest = p_starts[se] + jnp.arange(n_assign, dtype=jnp.int32) - starts[se]
    n_blocks = (n_assign + N_EXPERTS * (MOE_BLOCK - 1) + MOE_BLOCK - 1) // MOE_BLOCK
    slots = n_blocks * MOE_BLOCK
    buf_tok = jnp.zeros((slots,), jnp.int32).at[dest].set(flat_tok[order])
    buf_gate = jnp.zeros((slots,), jnp.float32).at[dest].set(flat_gate[order])
    blk_start = jnp.arange(n_blocks, dtype=jnp.int32) * MOE_BLOCK
    blk_expert = jnp.minimum(jnp.searchsorted(p_ends, blk_start, side='right'), N_EXPERTS - 1)

    def expert_block(args):
        e, tok, g = args
        xb = h[tok]
        a = xb @ w_gate[e]
        b = xb @ w_up[e]
        yb = (jax.nn.silu(a) * b) @ w_down[e]
        return yb * g[:, None].astype(yb.dtype)

    ys = lax.map(expert_block, (blk_expert, buf_tok.reshape(n_blocks, MOE_BLOCK), buf_gate.reshape(n_blocks, MOE_BLOCK)))
    return jax.ops.segment_sum(ys.reshape(slots, -1), buf_tok, num_segments=T)


def setup_inputs(seed: int = 0) -> dict:
    key = jax.random.key(seed)
    ks = jax.random.split(key, 19)
    D = D_MODEL
    f32 = jnp.float32

    def nrm(k, shape, scale):
        return jax.random.normal(k, shape, f32) * scale

    return {
        'x': nrm(ks[0], (BATCH, SEQ, D), 1.0),
        'c': nrm(ks[1], (BATCH, D), 1.0),
        'ctx': nrm(ks[2], (BATCH, CTX_LEN, D), 1.0),
        'c_ctx': nrm(ks[3], (D,), 1.0),
        'w_mod': nrm(ks[4], (DEPTH, D, 6 * D), 0.5 * D ** -0.5),
        'b_mod': nrm(ks[5], (DEPTH, 6 * D), 0.02),
        'ln_g': 1.0 + nrm(ks[6], (DEPTH, 2, D), 0.02),
        'ln_b': nrm(ks[7], (DEPTH, 2, D), 0.02),
        'w_qkv': nrm(ks[8], (N_ATTN_LAYERS, D, QKV_DIM), D ** -0.5),
        'q_gain': 1.0 + nrm(ks[9], (N_ATTN_LAYERS, HEAD_DIM), 0.02),
        'k_gain': 1.0 + nrm(ks[10], (N_ATTN_LAYERS, HEAD_DIM), 0.02),
        'w_o': nrm(ks[11], (N_ATTN_LAYERS, D, D), DEEPNORM_BETA * D ** -0.5),
        'w_pool': nrm(ks[12], (N_POOL_LAYERS, N_POOL_GROUPS, POOL_GROUP_DIM, POOL_GROUP_DIM), DEEPNORM_BETA * POOL_GROUP_DIM ** -0.5),
        'pool_scale': 1.0 + nrm(ks[13], (N_POOL_LAYERS, D), 0.02),
        'w_router': nrm(ks[14], (D, N_EXPERTS), D ** -0.5),
        'router_bias': nrm(ks[15], (N_EXPERTS,), 0.01),
        'w_gate': nrm(ks[16], (DEPTH, N_EXPERTS, D, D_EXPERT), D ** -0.5),
        'w_up': nrm(ks[17], (DEPTH, N_EXPERTS, D, D_EXPERT), D ** -0.5),
        'w_down': nrm(ks[18], (DEPTH, N_EXPERTS, D_EXPERT, D), DEEPNORM_BETA * D_EXPERT ** -0.5),
    }


def reference(x, c, ctx, c_ctx, w_mod, b_mod, ln_g, ln_b, w_qkv, q_gain, k_gain, w_o, w_pool, pool_scale, w_router, router_bias, w_gate, w_up, w_down):
    B, L, D = x.shape
    C = ctx.shape[1]
    n_rows = L // GRID_W
    rope_cos, rope_sin = axial_rope_tables(n_rows, x.dtype)
    xc = ctx
    s_lat = jax.nn.silu(c)
    s_ctx = jax.nn.silu(c_ctx)
    for i in range(DEPTH):
        need_ctx = i < DEPTH - 1
        mixer = i % N_MIXERS
        idx = i // N_MIXERS
        mod = (s_lat @ w_mod[i] + b_mod[i])[:, None, :]
        sh1, sc1, g1, sh2, sc2, g2 = jnp.split(mod, 6, axis=-1)
        u = modulate(x, sh1, sc1)
        if mixer == 0 or need_ctx:
            modc = s_ctx @ w_mod[i] + b_mod[i]
            csh1, csc1, cg1, csh2, csc2, cg2 = jnp.split(modc, 6, axis=-1)
            uc = modulate(xc, csh1, csc1)
        if mixer == 0:
            y, yc = attention_mixer(u, uc, rope_cos, rope_sin, w_qkv[idx], q_gain[idx], k_gain[idx], w_o[idx], need_ctx)
        else:
            y = pool_mixer(u, w_pool[idx], pool_scale[idx])
            yc = pool_mixer(uc, w_pool[idx], pool_scale[idx]) if need_ctx else None
        x = layer_norm(DEEPNORM_ALPHA * x + g1 * y, ln_g[i, 0], ln_b[i, 0])
        h = modulate(x, sh2, sc2).reshape(B * L, D)
        if need_ctx:
            xc = layer_norm(DEEPNORM_ALPHA * xc + cg1 * yc, ln_g[i, 0], ln_b[i, 0])
            hc = modulate(xc, csh2, csc2).reshape(B * C, D)
            f_all = moe_ffn(jnp.concatenate([hc, h], axis=0), w_router, router_bias, w_gate[i], w_up[i], w_down[i])
            fc = f_all[:B * C].reshape(B, C, D)
            f = f_all[B * C:].reshape(B, L, D)
            xc = layer_norm(DEEPNORM_ALPHA * xc + cg2 * fc, ln_g[i, 1], ln_b[i, 1])
        else:
            f = moe_ffn(h, w_router, router_bias, w_gate[i], w_up[i], w_down[i]).reshape(B, L, D)
        x = layer_norm(DEEPNORM_ALPHA * x + g2 * f, ln_g[i, 1], ln_b[i, 1])
    return x
```

```python
import os
import numpy as np
from contextlib import ExitStack
import concourse.bass as bass
import concourse.mybir as mybir
from concourse.bass_utils import run_bass_kernel_spmd

F32 = mybir.dt.float32
BF16 = mybir.dt.bfloat16
I32 = mybir.dt.int32
ALU = mybir.AluOpType
AF = mybir.ActivationFunctionType
AX = mybir.AxisListType

D = 2048
SEQ = 8192
CTX = 256
NKEY = SEQ + CTX
NKT = NKEY // 128
OWN = 2048
NT = 17
NTOK = NT * 128
NH = 16
NKV = 4
HD = 128
NE = 32
DE = 1024
NBLK = 68
ALPHA = float((2 * 2) ** 0.25)
LN_EPS = 1e-6
RMS_EPS = 1e-6
ATTN_SCALE = float(HD ** -0.5)
WINS = (2, 4, 8, 16)


class TR:
    def __init__(self, nc, ndq=24):
        self.nc = nc
        self.eng = {"pe": nc.tensor, "act": nc.scalar, "dve": nc.vector, "pool": nc.gpsimd, "sp": nc.sync}
        self.csem = {e: nc.alloc_semaphore(name=f"cs_{e}") for e in ("pe", "act", "dve", "pool")}
        self.cnt = {e: 0 for e in self.csem}
        self.dsem = {q: [nc.alloc_semaphore(name=f"ds_{q}{i}") for i in range(ndq)] for q in ("sp", "pool")}
        self.dval = {q: [0] * ndq for q in ("sp", "pool")}
        self.dpos = {"sp": 0, "pool": 0}
        self.seen = {e: {} for e in self.eng}
        self.lastw = {}
        self.readers = {}

    def _sem(self, tk):
        return self.csem[tk[1]] if tk[0] == "c" else self.dsem[tk[1]][tk[2]]

    def wait(self, E, tok):
        tk, val = tok
        if tk[0] == "c" and tk[1] == E and E == "pe":
            return
        if self.seen[E].get(tk, 0) >= val:
            return
        self.eng[E].wait_ge(self._sem(tk), val)
        self.seen[E][tk] = val

    def _deps(self, E, reads, writes):
        for k in reads:
            t = self.lastw.get(k)
            if t:
                self.wait(E, t)
        for k in writes:
            t = self.lastw.get(k)
            if t:
                self.wait(E, t)
            for tk, val in self.readers.get(k, {}).items():
                self.wait(E, (tk, val))

    def _rec(self, tok, reads, writes):
        tk, val = tok
        for k in reads:
            self.readers.setdefault(k, {})[tk] = val
        for k in writes:
            self.lastw[k] = tok
            self.readers[k] = {}

    def op(self, E, fn, reads=(), writes=()):
        self._deps(E, reads, writes)
        inst = fn()
        self.cnt[E] += 1
        inst.then_inc(self.csem[E], 1)
        self._rec((("c", E), self.cnt[E]), reads, writes)

    def dma(self, Q, fn, reads=(), writes=()):
        self._deps(Q, reads, writes)
        i = self.dpos[Q] % len(self.dsem[Q])
        self.dpos[Q] += 1
        tk = ("d", Q, i)
        if self.dval[Q][i] > 0:
            self.wait(Q, (tk, self.dval[Q][i]))
        inst = fn()
        self.dval[Q][i] += 16
        inst.then_inc(self.dsem[Q][i], 16)
        self._rec((tk, self.dval[Q][i]), reads, writes)

    def flush(self):
        for Q in ("sp", "pool"):
            for i, v in enumerate(self.dval[Q]):
                if v > 0:
                    self.wait("sp", (("d", Q, i), v))
        for e in self.csem:
            if self.cnt[e] > 0:
                self.wait("sp", (("c", e), self.cnt[e]))
        self.nc.all_engine_barrier()
        for E in self.seen:
            for Q in ("sp", "pool"):
                for i, v in enumerate(self.dval[Q]):
                    self.seen[E][("d", Q, i)] = v
            for e in self.csem:
                self.seen[E][("c", e)] = self.cnt[e]
        self.lastw = {}
        self.readers = {}


def build(stop_after=99, debug=False):
    nc = bass.Bass("TRN2", target_bir_lowering=False)
    tr = TR(nc)
    pe, act, dve, pool, sp = nc.tensor, nc.scalar, nc.vector, nc.gpsimd, nc.sync

    def din(name, shape, dt=F32):
        if os.environ.get("MOEONLY") and name not in ("xs", "w_gate", "w_up", "w_down", "sei_in", "stt_in", "ident", "iotap"):
            return nc.dram_tensor(name, list(shape), dt, kind="Internal").ap()
        return nc.dram_tensor(name, list(shape), dt, kind="ExternalInput").ap()

    def dscr(name, shape, dt):
        kind = "ExternalOutput" if (debug and name in os.environ.get("DBG", "").split(",")) else "Internal"
        return nc.dram_tensor(name, list(shape), dt, kind=kind).ap()

    keysrc = din("keysrc", [NKEY, D])
    xq = din("xq", [NTOK, D])
    cpad = din("cpad", [128, 16 * 33])
    w_mod = din("w_mod", [2, D, 6 * D])
    b_mod = din("b_mod", [2, 6 * D])
    ln_g = din("ln_g", [2, 2, D])
    ln_b = din("ln_b", [2, 2, D])
    w_qkv = din("w_qkv", [D, 3072])
    q_gain = din("q_gain", [128, 1])
    k_gain = din("k_gain", [128, 1])
    w_o = din("w_o", [D, D])
    w_pool = din("w_pool", [4, 512, 512])
    pool_scale = din("pool_scale", [1, D])
    w_router = din("w_router", [D, NE])
    router_bias = din("router_bias", [1, NE])
    MOEONLY = bool(os.environ.get("MOEONLY"))
    NEX = int(os.environ.get("MOE_NE", str(NE)))
    if stop_after >= 4:
        w_gate = din("w_gate", [2, NEX, D, DE])
        w_up = din("w_up", [2, NEX, D, DE])
        w_down = din("w_down", [2, NEX, DE, D])
    ident_in = din("ident", [128, 128])
    perm_in = din("perm", [128, 128])
    triu_in = din("triu", [128, 128])
    cosK = din("cosK", [128, SEQ])
    sinK = din("sinK", [128, SEQ])
    cosQ = din("cosQ", [128, NTOK])
    sinQ = din("sinQ", [128, NTOK])
    poolB = din("poolB", [128, 4 * 7 * 128])
    iotap = din("iotap", [128, 1])
    out = nc.dram_tensor("out", [OWN, D], F32, kind="ExternalOutput").ap()

    modrow = dscr("modrow", [2, 2, 6 * D], F32)
    KT = dscr("KT", [NKV, 128, NKEY], BF16)
    Vt = dscr("Vt", [NKEY, 512], BF16)
    QT = dscr("QT", [NH, 128, NTOK], BF16)
    OTs = dscr("OTs", [NH, 128, NTOK], BF16)
    x1s = dscr("x1s", [NTOK, D], F32)
    x2s = dscr("x2s", [NTOK, D], F32)
    hs = dscr("hs", [NTOK, D], BF16)
    xs = din("xs", [NBLK * 128, D], BF16) if MOEONLY else dscr("xs", [NBLK * 128, D], BF16)
    ys0 = dscr("ys0", [NBLK * 128, D], F32)
    ys1 = dscr("ys1", [NBLK * 128, D], F32)
    ys = [ys0, ys1]

    CH = 512

    def phase0():
        with ExitStack() as _st:
            cT = _st.enter_context(nc.sbuf_tensor("p0_c", [128, 16 * 33], F32))
            sT = _st.enter_context(nc.sbuf_tensor("p0_s", [128, 16 * 33], F32))
            wt = _st.enter_context(nc.sbuf_tensor("p0_w", [128, 2, 16, CH], F32))
            bt = _st.enter_context(nc.sbuf_tensor("p0_b", [33, 2, CH], F32))
            ot = _st.enter_context(nc.sbuf_tensor("p0_o", [33, 2, CH], F32))
            ps = _st.enter_context(nc.psum_tensor("p0_ps", [33, 2, CH], F32))
            tr.dma("sp", lambda: sp.dma_start(out=cT[:], in_=cpad), writes=["cT"])
            tr.op("act", lambda: act.activation(out=sT[:], in_=cT[:], func=AF.Silu), reads=["cT"], writes=["sT"])
            it = 0
            for i in range(2):
                wsrc = w_mod[i].rearrange("(k p) n -> p k n", p=128)
                for n in range(24):
                    s = it % 2
                    it += 1
                    tr.dma("sp", lambda: sp.dma_start(out=wt[:, s], in_=wsrc[:, :, n * CH:(n + 1) * CH]),
                           writes=[("wt", s)])
                    tr.dma("sp", lambda: sp.dma_start(out=bt[0:1, s, :], in_=b_mod[i:i + 1, n * CH:(n + 1) * CH]),
                           writes=[("bt0", s)])
                    tr.dma("sp", lambda: sp.dma_start(out=bt[32:33, s, :], in_=b_mod[i:i + 1, n * CH:(n + 1) * CH]),
                           writes=[("bt1", s)])
                    for k in range(16):
                        tr.op("pe", lambda: pe.matmul(ps[:, s, :], lhsT=sT[:, k * 33:(k + 1) * 33], rhs=wt[:, s, k, :],
                                                       start=(k == 0), stop=(k == 15)),
                              reads=["sT", ("wt", s)], writes=[("ps0", s)])
                    tr.op("dve", lambda: dve.tensor_tensor(out=ot[0:1, s, :], in0=ps[0:1, s, :], in1=bt[0:1, s, :], op=ALU.add),
                          reads=[("ps0", s), ("bt0", s)], writes=[("ot0", s)])
                    tr.op("dve", lambda: dve.tensor_tensor(out=ot[32:33, s, :], in0=ps[32:33, s, :], in1=bt[32:33, s, :], op=ALU.add),
                          reads=[("ps0", s), ("bt1", s)], writes=[("ot1", s)])
                    tr.dma("sp", lambda: sp.dma_start(out=modrow[i, 0:1, n * CH:(n + 1) * CH], in_=ot[0:1, s, :]),
                           reads=[("ot0", s)], writes=["modrow"])
                    tr.dma("sp", lambda: sp.dma_start(out=modrow[i, 1:2, n * CH:(n + 1) * CH], in_=ot[32:33, s, :]),
                           reads=[("ot1", s)], writes=["modrow"])
            tr.flush()

    def mod_bcast(t, layer, which, idx):
        tr.dma("sp", lambda: sp.dma_start(out=t[:], in_=modrow[layer, which:which + 1, idx * D:(idx + 1) * D].partition_broadcast(128)),
               reads=["modrow"], writes=[t.name])

    def mod_col(t, layer, which, idx):
        with nc.allow_non_contiguous_dma("tiny column load"):
            tr.dma("sp", lambda: sp.dma_start(out=t[:], in_=modrow[layer, which, idx * D:(idx + 1) * D].rearrange("(k p) -> p k", p=128)),
                   reads=["modrow"], writes=[t.name])

    def phase1():
        with ExitStack() as _st:
            wkv = _st.enter_context(nc.sbuf_tensor("p1_wkv", [128, 16, 1024], BF16))
            wq = _st.enter_context(nc.sbuf_tensor("p1_wq", [128, 16, 2048], BF16))
            ident = _st.enter_context(nc.sbuf_tensor("p1_id", [128, 128], F32))
            perm = _st.enter_context(nc.sbuf_tensor("p1_pm", [128, 128], BF16))
            ones = _st.enter_context(nc.sbuf_tensor("p1_on", [128, 128], BF16))
            epst = _st.enter_context(nc.sbuf_tensor("p1_eps", [128, 1], F32))
            gq = _st.enter_context(nc.sbuf_tensor("p1_gq", [128, 1], F32))
            gk = _st.enter_context(nc.sbuf_tensor("p1_gk", [128, 1], F32))
            shl = _st.enter_context(nc.sbuf_tensor("p1_shl", [128, 16], F32))
            scl = _st.enter_context(nc.sbuf_tensor("p1_scl", [128, 16], F32))
            shc = _st.enter_context(nc.sbuf_tensor("p1_shc", [128, 16], F32))
            scc = _st.enter_context(nc.sbuf_tensor("p1_scc", [128, 16], F32))
            xt = _st.enter_context(nc.sbuf_tensor("p1_x", [128, 2, D], F32))
            uT = _st.enter_context(nc.sbuf_tensor("p1_u", [128, 16, CH], BF16))
            cost = _st.enter_context(nc.sbuf_tensor("p1_cos", [128, 2, CH], F32))
            sint = _st.enter_context(nc.sbuf_tensor("p1_sin", [128, 2, CH], F32))
            sq = _st.enter_context(nc.sbuf_tensor("p1_sq", [128, 2, CH], BF16))
            kg = _st.enter_context(nc.sbuf_tensor("p1_kg", [128, 2, CH], BF16))
            rs = _st.enter_context(nc.sbuf_tensor("p1_rs", [128, 2, CH], F32))
            t1 = _st.enter_context(nc.sbuf_tensor("p1_t1", [128, 2, CH], F32))
            t2 = _st.enter_context(nc.sbuf_tensor("p1_t2", [128, 2, CH], F32))
            ko = _st.enter_context(nc.sbuf_tensor("p1_ko", [128, 2, CH], BF16))
            vo = _st.enter_context(nc.sbuf_tensor("p1_vo", [128, 2, 512], BF16))
            pT = _st.enter_context(nc.psum_tensor("p1_pT", [128, 2, 4, 128], F32))
            pA = _st.enter_context(nc.psum_tensor("p1_pA", [128, 2, CH], F32))
            pB = _st.enter_context(nc.psum_tensor("p1_pB", [128, 2, CH], F32))
            pC = _st.enter_context(nc.psum_tensor("p1_pC", [128, 2, CH], F32))
            wsrc = w_qkv.rearrange("(k p) n -> p k n", p=128)
            for k4 in range(4):
                tr.dma("pool", lambda: pool.dma_start(out=wkv[:, k4 * 4:(k4 + 1) * 4, :], in_=wsrc[:, k4 * 4:(k4 + 1) * 4, 2048:3072]),
                       writes=[wkv.name])
            for k4 in range(8):
                for hh_ in range(2):
                    tr.dma("pool", lambda: pool.dma_start(out=wq[:, k4 * 2:(k4 + 1) * 2, hh_ * 1024:(hh_ + 1) * 1024],
                                                          in_=wsrc[:, k4 * 2:(k4 + 1) * 2, hh_ * 1024:(hh_ + 1) * 1024]),
                           writes=[wq.name])
            tr.dma("sp", lambda: sp.dma_start(out=ident[:], in_=ident_in), writes=["ident"])
            tr.dma("pool", lambda: pool.dma_start(out=perm[:], in_=perm_in), writes=["perm"])
            tr.dma("sp", lambda: sp.dma_start(out=gq[:], in_=q_gain), writes=[gq.name])
            tr.dma("sp", lambda: sp.dma_start(out=gk[:], in_=k_gain), writes=[gk.name])
            tr.op("dve", lambda: dve.memset(ones[:], 1.0), writes=["ones"])
            tr.op("dve", lambda: dve.memset(epst[:], RMS_EPS), writes=["eps"])
            mod_col(shl, 0, 0, 0)
            mod_col(scl, 0, 0, 1)
            mod_col(shc, 0, 1, 0)
            mod_col(scc, 0, 1, 1)
            tr.op("dve", lambda: dve.tensor_scalar(out=scl[:], in0=scl[:], scalar1=1.0, scalar2=None, op0=ALU.add),
                  reads=[scl.name], writes=[scl.name])
            tr.op("dve", lambda: dve.tensor_scalar(out=scc[:], in0=scc[:], scalar1=1.0, scalar2=None, op0=ALU.add),
                  reads=[scc.name], writes=[scc.name])

            cnt = {"x": 0, "q": 0}

            def load_uT(src, row0, ntile, sh, sc):
                for t in range(ntile):
                    s = cnt["x"] % 2
                    cnt["x"] += 1
                    tr.dma("sp", lambda: sp.dma_start(out=xt[:, s, :], in_=src[row0 + t * 128: row0 + (t + 1) * 128, :]),
                           writes=[("xt", s)])
                    for g in range(4):
                        b = g % 2
                        for kk in range(4):
                            k = g * 4 + kk
                            tr.op("pe", lambda: pe.transpose(pT[:, b, kk, :], xt[:, s, k * 128:(k + 1) * 128], ident[:]),
                                  reads=[("xt", s), "ident"], writes=[("pT", b)])
                        for kk in range(4):
                            k = g * 4 + kk
                            if os.environ.get("NOMOD"):
                                tr.op("dve", lambda: dve.tensor_copy(out=uT[:, k, t * 128:(t + 1) * 128], in_=pT[:, b, kk, :]),
                                      reads=[("pT", b)], writes=["uT"])
                            elif kk % 2 == 0:
                                tr.op("act", lambda: act.activation(out=uT[:, k, t * 128:(t + 1) * 128], in_=pT[:, b, kk, :],
                                                                    func=AF.Identity, scale=sc[:, k:k + 1], bias=sh[:, k:k + 1]),
                                      reads=[("pT", b), sc.name, sh.name], writes=["uT"])
                            else:
                                tr.op("dve", lambda: dve.tensor_scalar(out=uT[:, k, t * 128:(t + 1) * 128], in0=pT[:, b, kk, :],
                                                                       scalar1=sc[:, k:k + 1], scalar2=sh[:, k:k + 1],
                                                                       op0=ALU.mult, op1=ALU.add),
                                      reads=[("pT", b), sc.name, sh.name], writes=["uT"])

            def proj_T(w, col0, n, gain, cos_src, sin_src, pos0, dst):
                pc = int(os.environ.get("PCUT", "9"))
                s = cnt["q"] % 2
                cnt["q"] += 1
                for k in range(16):
                    tr.op("pe", lambda: pe.matmul(pA[:, s, :n], lhsT=w[:, k, col0:col0 + 128], rhs=uT[:, k, :n],
                                                   start=(k == 0), stop=(k == 15)),
                          reads=["uT", w.name], writes=[("pA", s)])
                if pc < 2:
                    return
                tr.op("act", lambda: act.activation(out=sq[:, s, :n], in_=pA[:, s, :n], func=AF.Square),
                      reads=[("pA", s)], writes=[("sq", s)])
                if pc < 3:
                    return
                if os.environ.get("KGACT", "1") == "1":
                    tr.op("act", lambda: act.activation(out=kg[:, s, :n], in_=pA[:, s, :n], func=AF.Copy, scale=gain[:, 0:1]),
                          reads=[("pA", s), gain.name], writes=[("kg", s)])
                else:
                    tr.op("dve", lambda: dve.tensor_scalar(out=kg[:, s, :n], in0=pA[:, s, :n], scalar1=gain[:, 0:1], scalar2=None, op0=ALU.mult),
                          reads=[("pA", s), gain.name], writes=[("kg", s)])
                if pc < 4:
                    return
                tr.op("pe", lambda: pe.matmul(pB[:, s, :n], lhsT=ones[:], rhs=sq[:, s, :n], start=True, stop=True),
                      reads=[("sq", s), "ones"], writes=[("pB", s)])
                if pc < 5:
                    return
                tr.op("dve", lambda: dve.tensor_scalar(out=rs[:, s, :n], in0=pB[:, s, :n], scalar1=1.0 / HD, scalar2=RMS_EPS, op0=ALU.mult, op1=ALU.add),
                      reads=[("pB", s)], writes=[("rs", s)])
                tr.op("act", lambda: act.activation(out=rs[:, s, :n], in_=rs[:, s, :n], func=AF.Sqrt),
                      reads=[("rs", s)], writes=[("rs", s)])
                if pc < 6:
                    return
                tr.op("dve", lambda: dve.reciprocal(out=rs[:, s, :n], in_=rs[:, s, :n]), reads=[("rs", s)], writes=[("rs", s)])
                if pc < 7:
                    return
                if cos_src is not None:
                    tr.op("pe", lambda: pe.matmul(pC[:, s, :n], lhsT=perm[:], rhs=kg[:, s, :n], start=True, stop=True),
                          reads=[("kg", s), "perm"], writes=[("pC", s)])
                    tr.dma("sp", lambda: sp.dma_start(out=cost[:, s, :n], in_=cos_src[:, pos0:pos0 + n]), writes=[("cos", s)])
                    tr.dma("sp", lambda: sp.dma_start(out=sint[:, s, :n], in_=sin_src[:, pos0:pos0 + n]), writes=[("sin", s)])
                    tr.op("dve", lambda: dve.tensor_tensor(out=t1[:, s, :n], in0=kg[:, s, :n], in1=cost[:, s, :n], op=ALU.mult),
                          reads=[("kg", s), ("cos", s)], writes=[("t1", s)])
                    tr.op("act", lambda: act.copy(out=t2[:, s, :n], in_=pC[:, s, :n]), reads=[("pC", s)], writes=[("t2", s)])
                    tr.op("dve", lambda: dve.tensor_tensor(out=t2[:, s, :n], in0=t2[:, s, :n], in1=sint[:, s, :n], op=ALU.mult),
                          reads=[("t2", s), ("sin", s)], writes=[("t2", s)])
                    tr.op("dve", lambda: dve.tensor_tensor(out=t1[:, s, :n], in0=t1[:, s, :n], in1=t2[:, s, :n], op=ALU.add),
                          reads=[("t1", s), ("t2", s)], writes=[("t1", s)])
                    tr.op("dve", lambda: dve.tensor_tensor(out=ko[:, s, :n], in0=t1[:, s, :n], in1=rs[:, s, :n], op=ALU.mult),
                          reads=[("t1", s), ("rs", s)], writes=[("ko", s)])
                else:
                    tr.op("dve", lambda: dve.tensor_tensor(out=ko[:, s, :n], in0=kg[:, s, :n], in1=rs[:, s, :n], op=ALU.mult),
                          reads=[("kg", s), ("rs", s)], writes=[("ko", s)])
                if pc < 8:
                    return
                tr.dma("sp", lambda: sp.dma_start(out=dst, in_=ko[:, s, :n]), reads=[("ko", s)], writes=["KTQT"])

            cut = int(os.environ.get("K1CUT", "9"))
            chunks = [(0, 256, True)] + [(CTX + i * CH, CH, False) for i in range(SEQ // CH)]
            if cut == 0:
                chunks = []
            elif cut == 1:
                chunks = chunks[:1]
            elif cut == 2:
                chunks = chunks[:2]
            for (k0, n, is_ctx) in chunks:
                load_uT(keysrc, k0, n // 128, shc if is_ctx else shl, scc if is_ctx else scl)
                for j in range(NKV if not os.environ.get("SKIPK") else 0):
                    proj_T(wkv, j * 128, n, gk, None if is_ctx else cosK, None if is_ctx else sinK,
                           k0 - CTX, KT[j, :, k0:k0 + n])
                for t in range(n // 128 if not os.environ.get("SKIPV") else 0):
                    s = cnt["q"] % 2
                    cnt["q"] += 1
                    for k in range(16):
                        tr.op("pe", lambda: pe.matmul(pA[:, s, :], lhsT=uT[:, k, t * 128:(t + 1) * 128], rhs=wkv[:, k, 512:1024],
                                                       start=(k == 0), stop=(k == 15)),
                              reads=["uT", wkv.name], writes=[("pA", s)])
                    tr.op("act", lambda: act.copy(out=vo[:, s, :], in_=pA[:, s, :]), reads=[("pA", s)], writes=[("vo", s)])
                    tr.dma("sp", lambda: sp.dma_start(out=Vt[k0 + t * 128:k0 + (t + 1) * 128, :], in_=vo[:, s, :]),
                           reads=[("vo", s)], writes=["Vt"])
            for (q0, n) in ([(i * CH, CH) for i in range(4)] + [(2048, 128)] if cut >= 9 else []):
                load_uT(xq, q0, n // 128, shl, scl)
                for h in range(NH):
                    proj_T(wq, h * 128, n, gq, cosQ, sinQ, q0, QT[h, :, q0:q0 + n])
            tr.flush()

    def phase2():
        with ExitStack() as _st:
            Ks = _st.enter_context(nc.sbuf_tensor("p2_K", [128, NKV, NKEY], BF16))
            Vs = _st.enter_context(nc.sbuf_tensor("p2_V", [128, NKT, 512], BF16))
            ones = _st.enter_context(nc.sbuf_tensor("p2_on", [128, 128], BF16))
            qt = _st.enter_context(nc.sbuf_tensor("p2_q", [128, 2, CH], BF16))
            pt = _st.enter_context(nc.sbuf_tensor("p2_p", [128, 3, CH], BF16))
            rd = _st.enter_context(nc.sbuf_tensor("p2_rd", [128, 2, CH], F32))
            ot = _st.enter_context(nc.sbuf_tensor("p2_o", [128, 2, CH], BF16))
            pS = _st.enter_context(nc.psum_tensor("p2_pS", [128, 3, CH], F32))
            pO = _st.enter_context(nc.psum_tensor("p2_pO", [128, 2, CH], F32))
            pD = _st.enter_context(nc.psum_tensor("p2_pD", [128, 2, CH], F32))
            tr.op("dve", lambda: dve.memset(ones[:], 1.0), writes=["ones"])
            for j in range(NKV):
                for c in range(4):
                    c0, c1 = c * 2112, (c + 1) * 2112
                    tr.dma("sp", lambda: sp.dma_start(out=Ks[:, j, c0:c1], in_=KT[j, :, c0:c1]), writes=["Ks"])
            vsrc = Vt.rearrange("(t p) n -> p t n", p=128)
            for c in range(6):
                tr.dma("sp", lambda: sp.dma_start(out=Vs[:, c * 11:(c + 1) * 11, :], in_=vsrc[:, c * 11:(c + 1) * 11, :]), writes=["Vs"])
            it = 0
            sctr = 0
            for h in range(NH):
                j = h // 4
                for (q0, n) in [(i * CH, CH) for i in range(4)] + [(2048, 128)]:
                    b = it % 2
                    it += 1
                    tr.dma("sp", lambda: sp.dma_start(out=qt[:, b, :n], in_=QT[h, :, q0:q0 + n]), writes=[("qt", b)])

                    def smm(kt, s):
                        tr.op("pe", lambda: pe.matmul(pS[:, s, :n], lhsT=Ks[:, j, kt * 128:(kt + 1) * 128], rhs=qt[:, b, :n],
                                                       start=True, stop=True),
                              reads=["Ks", ("qt", b)], writes=[("pS", s)])
                        tr.op("act", lambda: act.activation(out=pt[:, s, :n], in_=pS[:, s, :n], func=AF.Exp, scale=ATTN_SCALE),
                              reads=[("pS", s)], writes=[("pt", s)])

                    base = sctr
                    smm(0, base % 3)
                    smm(1, (base + 1) % 3)
                    for kt in range(NKT):
                        s = (base + kt) % 3
                        if kt + 2 < NKT:
                            smm(kt + 2, (base + kt + 2) % 3)
                        tr.op("pe", lambda: pe.matmul(pO[:, b, :n], lhsT=Vs[:, kt, j * 128:(j + 1) * 128], rhs=pt[:, s, :n],
                                                       start=(kt == 0), stop=(kt == NKT - 1)),
                              reads=["Vs", ("pt", s)], writes=[("pO", b)])
                        tr.op("pe", lambda: pe.matmul(pD[:, b, :n], lhsT=ones[:], rhs=pt[:, s, :n],
                                                       start=(kt == 0), stop=(kt == NKT - 1)),
                              reads=["ones", ("pt", s)], writes=[("pD", b)])
                    sctr += NKT
                    tr.op("dve", lambda: dve.reciprocal(out=rd[:, b, :n], in_=pD[:, b, :n]), reads=[("pD", b)], writes=[("rd", b)])
                    tr.op("dve", lambda: dve.tensor_tensor(out=ot[:, b, :n], in0=pO[:, b, :n], in1=rd[:, b, :n], op=ALU.mult),
                          reads=[("pO", b), ("rd", b)], writes=[("ot", b)])
                    tr.dma("sp", lambda: sp.dma_start(out=OTs[h, :, q0:q0 + n], in_=ot[:, b, :n]), reads=[("ot", b)], writes=["OTs"])
            tr.flush()

    class RouterState:
        pass

    def alloc_router(stack, tstack, layer, ntile):
        R = RouterState()
        R.layer = layer
        R.ntile = ntile
        e = stack.enter_context
        te = tstack.enter_context
        R.epst = e(nc.sbuf_tensor(f"r{layer}_eps", [128, 1], F32))
        R.carry = e(nc.sbuf_tensor(f"r{layer}_cy", [128, NE], F32))
        R.A = e(nc.sbuf_tensor(f"r{layer}_A", [128, NT, NE], F32))
        R.pos = e(nc.sbuf_tensor(f"r{layer}_pos", [128, NT, NE], F32))
        R.gt = e(nc.sbuf_tensor(f"r{layer}_gt", [128, NT, NE], F32))
        R.st = e(nc.sbuf_tensor(f"r{layer}_st", [128, 4, 6], F32))
        R.mv = e(nc.sbuf_tensor(f"r{layer}_mv", [128, 2], F32))
        R.rstd = e(nc.sbuf_tensor(f"r{layer}_rstd", [128, 1], F32))
        R.lng = te(nc.sbuf_tensor(f"r{layer}_lng", [128, D], F32))
        R.lnb = te(nc.sbuf_tensor(f"r{layer}_lnb", [128, D], F32))
        R.scp = te(nc.sbuf_tensor(f"r{layer}_scp", [128, D], F32))
        R.shb = te(nc.sbuf_tensor(f"r{layer}_shb", [128, D], F32))
        R.wr = te(nc.sbuf_tensor(f"r{layer}_wr", [128, 16, NE], F32))
        R.rb = te(nc.sbuf_tensor(f"r{layer}_rb", [128, NE], F32))
        R.ident = te(nc.sbuf_tensor(f"r{layer}_id", [128, 128], F32))
        R.triu = te(nc.sbuf_tensor(f"r{layer}_tu", [128, 128], F32))
        R.onesf = te(nc.sbuf_tensor(f"r{layer}_on", [128, 128], F32))
        R.x1 = te(nc.sbuf_tensor(f"r{layer}_x1", [128, D], F32))
        R.hh = te(nc.sbuf_tensor(f"r{layer}_hh", [128, D], F32))
        R.hb = te(nc.sbuf_tensor(f"r{layer}_hb", [128, D], BF16))
        R.hT = te(nc.sbuf_tensor(f"r{layer}_hT", [128, 16, 128], F32))
        R.sm = te(nc.sbuf_tensor(f"r{layer}_sm", [128, 12, NE], F32))
        R.ext = te(nc.sbuf_tensor(f"r{layer}_ext", [128, 8, 8], F32))
        R.g8 = te(nc.sbuf_tensor(f"r{layer}_g8", [128, 8, 8], F32))
        R.s1 = te(nc.sbuf_tensor(f"r{layer}_s1", [128, 4], F32))
        R.pTr = te(nc.psum_tensor(f"r{layer}_pTr", [128, 4, 4, 128], F32))
        R.pSm = te(nc.psum_tensor(f"r{layer}_pSm", [128, 4, NE], F32))
        tr.dma("sp", lambda: sp.dma_start(out=R.lng[:], in_=ln_g[layer, 0:1, :].partition_broadcast(128)), writes=[R.lng.name])
        tr.dma("sp", lambda: sp.dma_start(out=R.lnb[:], in_=ln_b[layer, 0:1, :].partition_broadcast(128)), writes=[R.lnb.name])
        mod_bcast(R.scp, layer, 0, 4)
        mod_bcast(R.shb, layer, 0, 3)
        tr.op("pool", lambda: pool.tensor_scalar(out=R.scp[:], in0=R.scp[:], scalar1=1.0, scalar2=None, op0=ALU.add),
              reads=[R.scp.name], writes=[R.scp.name])
        tr.dma("sp", lambda: sp.dma_start(out=R.wr[:], in_=w_router.rearrange("(k p) e -> p k e", p=128)), writes=["wr"])
        tr.dma("sp", lambda: sp.dma_start(out=R.rb[:], in_=router_bias.partition_broadcast(128)), writes=["rb"])
        tr.dma("sp", lambda: sp.dma_start(out=R.ident[:], in_=ident_in), writes=["identr"])
        tr.dma("sp", lambda: sp.dma_start(out=R.triu[:], in_=triu_in), writes=["triu"])
        tr.op("dve", lambda: dve.memset(R.onesf[:], 1.0), writes=["onesf"])
        tr.op("dve", lambda: dve.memset(R.epst[:], LN_EPS), writes=["epsr"])
        tr.op("dve", lambda: dve.memset(R.carry[:], 0.0), writes=["carry"])
        return R

    def layer_norm(R, z, zkey, g, b, outt, outkey):
        for c in range(4):
            tr.op("dve", lambda: dve.bn_stats(out=R.st[:, c, :], in_=z[:, c * 512:(c + 1) * 512]), reads=[zkey], writes=["st"])
        tr.op("dve", lambda: dve.bn_aggr(out=R.mv[:], in_=R.st[:]), reads=["st"], writes=["mv"])
        tr.op("act", lambda: act.activation(out=R.rstd[:], in_=R.mv[:, 1:2], func=AF.Sqrt, scale=1.0, bias=R.epst[:, 0:1]),
              reads=["mv", "epsr"], writes=["rstd"])
        tr.op("dve", lambda: dve.reciprocal(out=R.rstd[:], in_=R.rstd[:]), reads=["rstd"], writes=["rstd"])
        tr.op("dve", lambda: dve.tensor_scalar(out=z[:], in0=z[:], scalar1=R.mv[:, 0:1], scalar2=R.rstd[:, 0:1],
                                               op0=ALU.subtract, op1=ALU.mult),
              reads=[zkey, "mv", "rstd"], writes=[zkey])
        tr.op("pool", lambda: pool.tensor_tensor(out=z[:], in0=z[:], in1=g[:], op=ALU.mult), reads=[zkey, g.name], writes=[zkey])
        tr.op("dve", lambda: dve.tensor_tensor(out=outt[:], in0=z[:], in1=b[:], op=ALU.add), reads=[zkey, b.name], writes=[outkey])

    def ln_router(R, ti, z, zkey, xdst):
        layer_norm(R, z, zkey, R.lng, R.lnb, R.x1, "x1")
        tr.dma("sp", lambda: sp.dma_start(out=xdst[ti * 128:(ti + 1) * 128, :], in_=R.x1[:]), reads=["x1"], writes=["xdst"])
        tr.op("pool", lambda: pool.tensor_tensor(out=R.hh[:], in0=R.x1[:], in1=R.scp[:], op=ALU.mult),
              reads=["x1", R.scp.name], writes=["hh"])
        tr.op("dve", lambda: dve.tensor_tensor(out=R.hh[:], in0=R.hh[:], in1=R.shb[:], op=ALU.add),
              reads=["hh", R.shb.name], writes=["hh"])
        tr.op("act", lambda: act.copy(out=R.hb[:], in_=R.hh[:]), reads=["hh"], writes=["hb"])
        tr.dma("sp", lambda: sp.dma_start(out=hs[ti * 128:(ti + 1) * 128, :], in_=R.hb[:]), reads=["hb"], writes=["hs"])
        for g in range(4):
            for kk in range(4):
                k = g * 4 + kk
                tr.op("pe", lambda: pe.transpose(R.pTr[:, g, kk, :], R.hh[:, k * 128:(k + 1) * 128], R.ident[:]),
                      reads=["hh", "identr"], writes=[("pTr", g)])
            if g % 2 == 0:
                tr.op("act", lambda: act.copy(out=R.hT[:, g * 4:(g + 1) * 4, :], in_=R.pTr[:, g, :, :]),
                      reads=[("pTr", g)], writes=["hT"])
            else:
                tr.op("dve", lambda: dve.tensor_copy(out=R.hT[:, g * 4:(g + 1) * 4, :], in_=R.pTr[:, g, :, :]),
                      reads=[("pTr", g)], writes=["hT"])
        for k in range(16):
            tr.op("pe", lambda: pe.matmul(R.pSm[:, 0, :], lhsT=R.hT[:, k, :], rhs=R.wr[:, k, :], start=(k == 0), stop=(k == 15)),
                  reads=["hT", "wr"], writes=["pLog"])
        sm = R.sm
        AFF, SEL, CNTR, SELM, GM, T0, T1 = (sm[:, i, :] for i in range(7))
        k_sm = "sm"
        tr.op("act", lambda: act.activation(out=AFF, in_=R.pSm[:, 0, :], func=AF.Sigmoid), reads=["pLog"], writes=[k_sm])
        tr.op("dve", lambda: dve.tensor_tensor(out=SEL, in0=AFF, in1=R.rb[:], op=ALU.add), reads=[k_sm, "rb"], writes=[k_sm])
        sel3 = sm[:, 1, :].rearrange("p (g j) -> p g j", j=4)
        tr.op("dve", lambda: dve.tensor_copy(out=R.ext[:, :, 0:4], in_=sel3), reads=[k_sm], writes=["ext"])
        tr.op("dve", lambda: dve.tensor_copy(out=R.ext[:, :, 4:8], in_=sel3), reads=[k_sm], writes=["ext"])
        cn3 = sm[:, 2, :].rearrange("p (g j) -> p g j", j=4)
        t03 = sm[:, 5, :].rearrange("p (g j) -> p g j", j=4)
        tr.op("dve", lambda: dve.tensor_tensor(out=cn3, in0=R.ext[:, :, 1:5], in1=sel3, op=ALU.is_gt), reads=["ext", k_sm], writes=[k_sm])
        for sft in (2, 3):
            tr.op("dve", lambda: dve.tensor_tensor(out=t03, in0=R.ext[:, :, sft:sft + 4], in1=sel3, op=ALU.is_gt),
                  reads=["ext", k_sm], writes=[k_sm])
            tr.op("dve", lambda: dve.tensor_tensor(out=cn3, in0=cn3, in1=t03, op=ALU.add), reads=[k_sm], writes=[k_sm])
        tr.op("dve", lambda: dve.tensor_scalar(out=SELM, in0=CNTR, scalar1=1.5, scalar2=None, op0=ALU.is_lt), reads=[k_sm], writes=[k_sm])
        tr.op("dve", lambda: dve.tensor_tensor(out=T0, in0=SELM, in1=SEL, op=ALU.mult), reads=[k_sm], writes=[k_sm])
        tr.op("dve", lambda: dve.tensor_reduce(out=R.g8[:, :, 0], in_=t03, axis=AX.X, op=ALU.add), reads=[k_sm], writes=["g8"])
        tr.op("dve", lambda: dve.tensor_reduce(out=R.s1[:, 0:1], in_=R.g8[:, :, 0], axis=AX.X, op=ALU.max), reads=["g8"], writes=["s1"])
        tr.op("dve", lambda: dve.tensor_scalar(out=R.g8[:, :, 1], in0=R.g8[:, :, 0], scalar1=R.s1[:, 0:1], scalar2=None, op0=ALU.is_ge),
              reads=["g8", "s1"], writes=["g8"])
        a3 = R.A[:, ti, :].rearrange("p (g j) -> p g j", j=4)
        sm3 = sm[:, 3, :].rearrange("p (g j) -> p g j", j=4)
        for jj in range(4):
            tr.op("dve", lambda: dve.tensor_tensor(out=a3[:, :, jj], in0=sm3[:, :, jj], in1=R.g8[:, :, 1], op=ALU.mult),
                  reads=[k_sm, "g8"], writes=[("A", ti)])
        tr.op("dve", lambda: dve.tensor_tensor(out=T1, in0=R.A[:, ti, :], in1=AFF, op=ALU.mult), reads=[("A", ti), k_sm], writes=[k_sm])
        tr.op("dve", lambda: dve.tensor_reduce(out=R.s1[:, 1:2], in_=T1, axis=AX.X, op=ALU.add), reads=[k_sm], writes=["s1b"])
        tr.op("dve", lambda: dve.reciprocal(out=R.s1[:, 2:3], in_=R.s1[:, 1:2]), reads=["s1b"], writes=["s1c"])
        tr.op("dve", lambda: dve.tensor_scalar(out=R.gt[:, ti, :], in0=T1, scalar1=R.s1[:, 2:3], scalar2=None, op0=ALU.mult),
              reads=[k_sm, "s1c"], writes=[("gt", ti)])
        tr.op("pe", lambda: pe.matmul(R.pSm[:, 1, :], lhsT=R.triu[:], rhs=R.A[:, ti, :], start=True, stop=True),
              reads=["triu", ("A", ti)], writes=["pPos"])
        tr.op("pe", lambda: pe.matmul(R.pSm[:, 2, :], lhsT=R.onesf[:], rhs=R.A[:, ti, :], start=True, stop=True),
              reads=["onesf", ("A", ti)], writes=["pCnt"])
        tr.op("dve", lambda: dve.tensor_tensor(out=R.pos[:, ti, :], in0=R.pSm[:, 1, :], in1=R.carry[:], op=ALU.add),
              reads=["pPos", "carry"], writes=[("pos", ti)])
        tr.op("dve", lambda: dve.tensor_tensor(out=R.carry[:], in0=R.pSm[:, 2, :], in1=R.carry[:], op=ALU.add),
              reads=["pCnt", "carry"], writes=["carry"])

    def dispatch(R, stack):
        e = stack.enter_context
        L = R.layer
        nb = e(nc.sbuf_tensor(f"d{L}_nb", [128, NE], F32))
        sc = e(nc.sbuf_tensor(f"d{L}_sc", [128, 2, NE], F32))
        stt = e(nc.sbuf_tensor(f"d{L}_st", [128, NE], F32))
        se = e(nc.sbuf_tensor(f"d{L}_se", [128, 2 * NE], F32))
        sei = e(nc.sbuf_tensor(f"d{L}_sei", [128, 2 * NE], I32))
        dd = e(nc.sbuf_tensor(f"d{L}_dd", [128, 3, NE], F32))
        R.slA = e(nc.sbuf_tensor(f"d{L}_slA", [128, NT], F32))
        R.slB = e(nc.sbuf_tensor(f"d{L}_slB", [128, NT], F32))
        R.gA = e(nc.sbuf_tensor(f"d{L}_gA", [128, NT], F32))
        R.gB = e(nc.sbuf_tensor(f"d{L}_gB", [128, NT], F32))
        R.slAi = e(nc.sbuf_tensor(f"d{L}_slAi", [128, NT], I32))
        R.slBi = e(nc.sbuf_tensor(f"d{L}_slBi", [128, NT], I32))
        hbuf = e(nc.sbuf_tensor(f"d{L}_hb", [128, 2, D], BF16))
        tr.op("dve", lambda: dve.memset(nb[:], 1.0), writes=["nb"])
        for jn in range(1, 18):
            tr.op("dve", lambda: dve.scalar_tensor_tensor(out=nb[:], in0=R.carry[:], scalar=float(128 * jn), in1=nb[:],
                                                         op0=ALU.is_gt, op1=ALU.add),
                  reads=["carry", "nb"], writes=["nb"])
        tr.op("dve", lambda: dve.tensor_copy(out=sc[:, 0, :], in_=nb[:]), reads=["nb"], writes=[("sc", 0)])
        cur = 0
        for dlt in (1, 2, 4, 8, 16):
            nxt = 1 - cur
            tr.op("dve", lambda: dve.tensor_copy(out=sc[:, nxt, 0:dlt], in_=sc[:, cur, 0:dlt]), reads=[("sc", cur)], writes=[("sc", nxt)])
            tr.op("dve", lambda: dve.tensor_tensor(out=sc[:, nxt, dlt:NE], in0=sc[:, cur, dlt:NE], in1=sc[:, cur, 0:NE - dlt], op=ALU.add),
                  reads=[("sc", cur)], writes=[("sc", nxt)])
            cur = nxt
        tr.op("dve", lambda: dve.tensor_tensor(out=stt[:], in0=sc[:, cur, :], in1=nb[:], op=ALU.subtract),
              reads=[("sc", cur), "nb"], writes=["stt"])
        tr.op("dve", lambda: dve.tensor_copy(out=se[:, 0:NE], in_=stt[:]), reads=["stt"], writes=["se"])
        tr.op("dve", lambda: dve.tensor_copy(out=se[:, NE:2 * NE], in_=sc[:, cur, :]), reads=[("sc", cur)], writes=["se"])
        tr.op("dve", lambda: dve.tensor_copy(out=sei[:], in_=se[:]), reads=["se"], writes=["sei"])
        for ti in range(R.ntile):
            DEST, VA, VB = dd[:, 0, :], dd[:, 1, :], dd[:, 2, :]
            tr.op("dve", lambda: dve.scalar_tensor_tensor(out=DEST, in0=stt[:], scalar=128.0, in1=R.pos[:, ti, :], op0=ALU.mult, op1=ALU.add),
                  reads=["stt", ("pos", ti)], writes=["dd"])
            tr.op("dve", lambda: dve.scalar_tensor_tensor(out=VA, in0=DEST, scalar=1.0, in1=R.A[:, ti, :], op0=ALU.add, op1=ALU.mult),
                  reads=["dd", ("A", ti)], writes=["dd"])
            tr.op("dve", lambda: dve.tensor_reduce(out=R.slA[:, ti:ti + 1], in_=VA, axis=AX.X, op=ALU.max), reads=["dd"], writes=["slA"])
            tr.op("dve", lambda: dve.tensor_scalar(out=VB, in0=R.A[:, ti, :], scalar1=-1.0e6, scalar2=1.0e6, op0=ALU.mult, op1=ALU.add),
                  reads=[("A", ti)], writes=["dd"])
            tr.op("dve", lambda: dve.tensor_tensor(out=VB, in0=VB, in1=DEST, op=ALU.add), reads=["dd"], writes=["dd"])
            tr.op("dve", lambda: dve.tensor_reduce(out=R.slB[:, ti:ti + 1], in_=VB, axis=AX.X, op=ALU.min), reads=["dd"], writes=["slB"])
            tr.op("dve", lambda: dve.tensor_scalar(out=VA, in0=VA, scalar1=R.slA[:, ti:ti + 1], scalar2=None, op0=ALU.is_ge),
                  reads=["dd", "slA"], writes=["dd"])
            tr.op("dve", lambda: dve.tensor_tensor(out=VA, in0=VA, in1=R.gt[:, ti, :], op=ALU.mult), reads=["dd", ("gt", ti)], writes=["dd"])
            tr.op("dve", lambda: dve.tensor_reduce(out=R.gA[:, ti:ti + 1], in_=VA, axis=AX.X, op=ALU.add), reads=["dd"], writes=["gA"])
        tr.op("dve", lambda: dve.tensor_scalar(out=R.slA[:], in0=R.slA[:], scalar1=-1.0, scalar2=None, op0=ALU.add), reads=["slA"], writes=["slA"])
        tr.op("dve", lambda: dve.tensor_scalar(out=R.gB[:], in0=R.gA[:], scalar1=-1.0, scalar2=1.0, op0=ALU.mult, op1=ALU.add),
              reads=["gA"], writes=["gB"])
        tr.op("dve", lambda: dve.tensor_copy(out=R.slAi[:], in_=R.slA[:]), reads=["slA"], writes=["slAi"])
        tr.op("dve", lambda: dve.tensor_copy(out=R.slBi[:], in_=R.slB[:]), reads=["slB"], writes=["slBi"])
        for ti in range(R.ntile):
            s = ti % 2
            tr.dma("sp", lambda: sp.dma_start(out=hbuf[:, s, :], in_=hs[ti * 128:(ti + 1) * 128, :]), reads=["hs"], writes=[("hbuf", s)])
            tr.dma("pool", lambda: pool.indirect_dma_start(out=xs[:, :], out_offset=bass.IndirectOffsetOnAxis(ap=R.slAi[:, ti:ti + 1], axis=0),
                                                           in_=hbuf[:, s, :], in_offset=None),
                   reads=[("hbuf", s), "slAi"], writes=["xs"])
            tr.dma("pool", lambda: pool.indirect_dma_start(out=xs[:, :], out_offset=bass.IndirectOffsetOnAxis(ap=R.slBi[:, ti:ti + 1], axis=0),
                                                           in_=hbuf[:, s, :], in_offset=None),
                   reads=[("hbuf", s), "slBi"], writes=["xs"])
        tr.flush()
        R.sei = sei
        R.stt = stt
        return None

    def moe_experts(layer, R):
        with ExitStack() as _st:
            wg = _st.enter_context(nc.sbuf_tensor(f"m{layer}_wg", [128, 3, 16, 512], BF16))
            wu = _st.enter_context(nc.sbuf_tensor(f"m{layer}_wu", [128, 3, 16, 512], BF16))
            wd = _st.enter_context(nc.sbuf_tensor(f"m{layer}_wd", [128, 3, 4, D], BF16))
            identb = _st.enter_context(nc.sbuf_tensor(f"m{layer}_id", [128, 128], BF16))
            xb = _st.enter_context(nc.sbuf_tensor(f"m{layer}_xb", [128, D], BF16))
            xT = _st.enter_context(nc.sbuf_tensor(f"m{layer}_xT", [128, 16, 128], BF16))
            sa = _st.enter_context(nc.sbuf_tensor(f"m{layer}_sa", [128, 512], F32))
            hm = _st.enter_context(nc.sbuf_tensor(f"m{layer}_hm", [128, 512], BF16))
            hmT = _st.enter_context(nc.sbuf_tensor(f"m{layer}_hmT", [128, 4, 128], BF16))
            yb = _st.enter_context(nc.sbuf_tensor(f"m{layer}_yb", [128, D], F32))
            pT = _st.enter_context(nc.psum_tensor(f"m{layer}_pT", [128, 16, 128], BF16))
            pA = _st.enter_context(nc.psum_tensor(f"m{layer}_pA", [128, 512], F32))
            pB = _st.enter_context(nc.psum_tensor(f"m{layer}_pB", [128, 512], F32))
            pY = _st.enter_context(nc.psum_tensor(f"m{layer}_pY", [128, 4, 512], F32))
            L = [nc.alloc_semaphore(name=f"ml{layer}_{i}") for i in range(12)]
            ENG = mybir.ALL_ENGINES
            rst = nc.alloc_registers(f"rst{layer}", engines=ENG)
            ren = nc.alloc_registers(f"ren{layer}", engines=ENG)
            iot = _st.enter_context(nc.sbuf_tensor(f"m{layer}_iot", [128, 1], F32))
            idxf = _st.enter_context(nc.sbuf_tensor(f"m{layer}_idxf", [128, 1], F32))
            idxi = _st.enter_context(nc.sbuf_tensor(f"m{layer}_idxi", [128, 1], I32))
            tr.dma("pool", lambda: pool.dma_start(out=identb[:], in_=ident_in), writes=["identb"])
            tr.dma("sp", lambda: sp.dma_start(out=iot[:], in_=iotap), writes=["iot"])

            def load_unit(u):
                ex, hf = u // 2, u % 2
                s = u % 3
                gsrc = w_gate[layer, ex].rearrange("(k p) n -> p k n", p=128)
                usrc = w_up[layer, ex].rearrange("(k p) n -> p k n", p=128)
                dsrc = w_down[layer, ex].rearrange("(k p) n -> p k n", p=128)
                for q in range(4):
                    tr.dma("pool", lambda: pool.dma_start(out=wg[:, s, q * 4:(q + 1) * 4, :], in_=gsrc[:, q * 4:(q + 1) * 4, hf * 512:(hf + 1) * 512]),
                           writes=[("w", s)])
                    tr.dma("pool", lambda: pool.dma_start(out=wu[:, s, q * 4:(q + 1) * 4, :], in_=usrc[:, q * 4:(q + 1) * 4, hf * 512:(hf + 1) * 512]),
                           writes=[("w", s)])
                    for hh_ in range(2):
                        tr.dma("pool", lambda: pool.dma_start(out=wd[:, s, q:q + 1, hh_ * 1024:(hh_ + 1) * 1024],
                                                              in_=dsrc[:, hf * 4 + q:hf * 4 + q + 1, hh_ * 1024:(hh_ + 1) * 1024]),
                               writes=[("w", s)])

            NU = 2 * NEX
            tr.flush()
            for sm_ in L:
                pool.sem_clear(sm_)
            rwi = pe.alloc_register(f"rwi{layer}")
            rwo = pool.alloc_register(f"rwo{layer}")
            pe.reg_mov(rwi, 16)
            pool.reg_mov(rwo, 16)
            nc.all_engine_barrier()
            load_unit(0)
            load_unit(1)
            for u in range(NU):
                ex, hf = u // 2, u % 2
                s = u % 3
                if u + 2 < NU:
                    load_unit(u + 2)
                tr.op("dve", lambda: dve.scalar_tensor_tensor(out=idxf[:], in0=R.stt[:, ex:ex + 1], scalar=128.0, in1=iot[:], op0=ALU.mult, op1=ALU.add),
                      reads=["stt", "iot"], writes=["idxf"])
                tr.op("dve", lambda: dve.tensor_copy(out=idxi[:], in_=idxf[:]), reads=["idxf"], writes=["idxi"])
                tr.op("pe", lambda: pe.matmul(pA[:, 0:1], lhsT=wg[:, s, 0, 0:128], rhs=wd[:, s, 0, 0:1], start=True, stop=True),
                      reads=[("w", s), "identb"], writes=["pAdummy"])
                tr.wait("sp", (("c", "pe"), tr.cnt["pe"]))
                tr.wait("sp", (("c", "dve"), tr.cnt["dve"]))
                nc.all_engine_barrier()
                nc.regs_load(rst, R.sei[0:1, ex:ex + 1])
                nc.regs_load(ren, R.sei[0:1, NE + ex:NE + ex + 1])
                lid = nc.next_id()
                lstart, lend = f"moe_{lid}_loop", f"moe_{lid}_end"
                nc.br(lstart, engines=ENG)
                ydst = ys[hf]
                with nc.body(lstart, valid_engines=ENG):
                    mc = int(os.environ.get("MCUT", "9"))
                    if mc >= 1:
                        pool.indirect_dma_start(out=xb[:], out_offset=None, in_=xs[:, :],
                                                in_offset=bass.IndirectOffsetOnAxis(ap=idxi[:, 0:1], axis=0)).then_inc(L[0], 16)
                        pe.wait_ge(L[0], rwi)
                        pe.reg_add(rwi, rwi, 16)
                    if mc >= 2:
                        for k in range(16):
                            pe.transpose(pT[:, k, :], xb[:, k * 128:(k + 1) * 128], identb[:]).then_inc(L[1], 1)
                        dve.wait_ge(L[1], 8)
                        dve.tensor_copy(out=xT[:, 0:8, :], in_=pT[:, 0:8, :]).then_inc(L[2], 1)
                        act.wait_ge(L[1], 16)
                        act.copy(out=xT[:, 8:16, :], in_=pT[:, 8:16, :]).then_inc(L[2], 1)
                        pe.wait_ge(L[2], 2)
                    if mc >= 3:
                        for k in range(16):
                            mm = pe.matmul(pA[:], lhsT=xT[:, k, :], rhs=wg[:, s, k, :], start=(k == 0), stop=(k == 15))
                        mm.then_inc(L[3], 1)
                        for k in range(16):
                            mm = pe.matmul(pB[:], lhsT=xT[:, k, :], rhs=wu[:, s, k, :], start=(k == 0), stop=(k == 15))
                        mm.then_inc(L[3], 1)
                        act.wait_ge(L[3], 1)
                        act.activation(out=sa[:], in_=pA[:], func=AF.Silu).then_inc(L[4], 1)
                        dve.wait_ge(L[3], 2)
                        dve.wait_ge(L[4], 1)
                        dve.tensor_tensor(out=hm[:], in0=sa[:], in1=pB[:], op=ALU.mult).then_inc(L[5], 1)
                        pe.wait_ge(L[5], 1)
                    if mc >= 4:
                        for c in range(4):
                            pe.transpose(pT[:, c, :], hm[:, c * 128:(c + 1) * 128], identb[:]).then_inc(L[6], 1)
                        dve.wait_ge(L[6], 4)
                        dve.tensor_copy(out=hmT[:], in_=pT[:, 0:4, :]).then_inc(L[7], 1)
                        pe.wait_ge(L[7], 1)
                        for n in range(4):
                            for c in range(4):
                                mm = pe.matmul(pY[:, n, :], lhsT=hmT[:, c, :], rhs=wd[:, s, c, n * 512:(n + 1) * 512], start=(c == 0), stop=(c == 3))
                            mm.then_inc(L[8], 1)
                        act.wait_ge(L[8], 2)
                        act.copy(out=yb[:, 0:1024], in_=pY[:, 0:2, :]).then_inc(L[9], 1)
                        dve.wait_ge(L[8], 4)
                        dve.tensor_copy(out=yb[:, 1024:2048], in_=pY[:, 2:4, :]).then_inc(L[9], 1)
                        pool.wait_ge(L[9], 2)
                    if mc >= 5:
                        pool.indirect_dma_start(out=ydst[:, :], out_offset=bass.IndirectOffsetOnAxis(ap=idxi[:, 0:1], axis=0),
                                                in_=yb[:], in_offset=None).then_inc(L[10], 16)
                        pool.wait_ge(L[10], rwo)
                        pool.reg_add(rwo, rwo, 16)
                    if mc >= 6:
                        pool.tensor_scalar(out=idxf[:], in0=idxf[:], scalar1=128.0, scalar2=None, op0=ALU.add).then_inc(L[11], 1)
                        pool.wait_ge(L[11], 1)
                        pool.tensor_copy(out=idxi[:], in_=idxf[:]).then_inc(L[11], 1)
                        pool.wait_ge(L[11], 2)
                    nc.all_engine_barrier()
                    for si_, sm_ in enumerate(L):
                        if si_ in (0, 10):
                            continue
                        pool.sem_clear(sm_)
                    nc.all_engine_barrier()
                    nc.regs_alu(rst, rst, 1, op=ALU.add)
                    nc.br_lt(rst, ren, on_true=lstart, on_false=lend, engines=ENG)
                nc.switch_bb(lend)
            tr.flush()

    def combine(R, layer, ntile, xsrc, final):
        with ExitStack() as _st:
            g2b = _st.enter_context(nc.sbuf_tensor(f"c{layer}_g2", [128, D], F32))
            lng2 = _st.enter_context(nc.sbuf_tensor(f"c{layer}_lng", [128, D], F32))
            lnb2 = _st.enter_context(nc.sbuf_tensor(f"c{layer}_lnb", [128, D], F32))
            yy = _st.enter_context(nc.sbuf_tensor(f"c{layer}_y", [128, 4, D], F32))
            xx = _st.enter_context(nc.sbuf_tensor(f"c{layer}_x", [128, D], F32))
            oo = _st.enter_context(nc.sbuf_tensor(f"c{layer}_o", [128, D], F32))
            mod_bcast(g2b, layer, 0, 5)
            tr.dma("sp", lambda: sp.dma_start(out=lng2[:], in_=ln_g[layer, 1:2, :].partition_broadcast(128)), writes=[lng2.name])
            tr.dma("sp", lambda: sp.dma_start(out=lnb2[:], in_=ln_b[layer, 1:2, :].partition_broadcast(128)), writes=[lnb2.name])
            for ti in range(ntile):
                for q, (ysrc, sl) in enumerate([(ys0, R.slAi), (ys1, R.slAi), (ys0, R.slBi), (ys1, R.slBi)]):
                    tr.dma("pool", lambda: pool.indirect_dma_start(out=yy[:, q, :], out_offset=None, in_=ysrc[:, :],
                                                                   in_offset=bass.IndirectOffsetOnAxis(ap=sl[:, ti:ti + 1], axis=0)),
                           reads=["ys"], writes=[("yy", q)])
                tr.dma("sp", lambda: sp.dma_start(out=xx[:], in_=xsrc[ti * 128:(ti + 1) * 128, :]), writes=["xx"])
                tr.op("pool", lambda: pool.tensor_tensor(out=yy[:, 0, :], in0=yy[:, 0, :], in1=yy[:, 1, :], op=ALU.add),
                      reads=[("yy", 0), ("yy", 1)], writes=[("yy", 0)])
                tr.op("pool", lambda: pool.tensor_tensor(out=yy[:, 2, :], in0=yy[:, 2, :], in1=yy[:, 3, :], op=ALU.add),
                      reads=[("yy", 2), ("yy", 3)], writes=[("yy", 2)])
                tr.op("dve", lambda: dve.tensor_scalar(out=yy[:, 0, :], in0=yy[:, 0, :], scalar1=R.gA[:, ti:ti + 1], scalar2=None, op0=ALU.mult),
                      reads=[("yy", 0)], writes=[("yy", 0)])
                tr.op("dve", lambda: dve.scalar_tensor_tensor(out=yy[:, 0, :], in0=yy[:, 2, :], scalar=R.gB[:, ti:ti + 1], in1=yy[:, 0, :],
                                                             op0=ALU.mult, op1=ALU.add),
                      reads=[("yy", 0), ("yy", 2)], writes=[("yy", 0)])
                tr.op("dve", lambda: dve.tensor_tensor(out=yy[:, 0, :], in0=yy[:, 0, :], in1=g2b[:], op=ALU.mult),
                      reads=[("yy", 0), g2b.name], writes=[("yy", 0)])
                tr.op("dve", lambda: dve.scalar_tensor_tensor(out=xx[:], in0=xx[:], scalar=ALPHA, in1=yy[:, 0, :], op0=ALU.mult, op1=ALU.add),
                      reads=["xx", ("yy", 0)], writes=["xx"])
                layer_norm(R, xx, "xx", lng2, lnb2, oo, "oo")
                if final:
                    tr.dma("sp", lambda: sp.dma_start(out=out[ti * 128:(ti + 1) * 128, :], in_=oo[:]), reads=["oo"], writes=["out"])
                else:
                    tr.dma("sp", lambda: sp.dma_start(out=x2s[ti * 128:(ti + 1) * 128, :], in_=oo[:]), reads=["oo"], writes=["x2s"])
            tr.flush()


    def layer0_post():
        with ExitStack() as stack:
            with ExitStack() as st2:
                R = alloc_router(stack, st2, 0, NT)
                e = st2.enter_context
                wo = e(nc.sbuf_tensor("p3_wo", [128, 16, D], BF16))
                g1b = e(nc.sbuf_tensor("p3_g1", [128, D], F32))
                oT = e(nc.sbuf_tensor("p3_oT", [128, 2, 16, 128], BF16))
                xt = e(nc.sbuf_tensor("p3_x", [128, 2, D], F32))
                zt = e(nc.sbuf_tensor("p3_z", [128, D], F32))
                pY = R.pTr[:].rearrange("p a b c -> p a (b c)")
                wsrc = w_o.rearrange("(k p) n -> p k n", p=128)
                for k4 in range(8):
                    for hh_ in range(2):
                        tr.dma("pool", lambda: pool.dma_start(out=wo[:, k4 * 2:(k4 + 1) * 2, hh_ * 1024:(hh_ + 1) * 1024],
                                                              in_=wsrc[:, k4 * 2:(k4 + 1) * 2, hh_ * 1024:(hh_ + 1) * 1024]), writes=["wo"])
                mod_bcast(g1b, 0, 0, 2)
                for ti in range(NT):
                    s = ti % 2
                    tr.dma("sp", lambda: sp.dma_start(out=oT[:, s], in_=OTs[:, :, ti * 128:(ti + 1) * 128].rearrange("h p t -> p h t")),
                           writes=[("oT", s)])
                    tr.dma("sp", lambda: sp.dma_start(out=xt[:, s, :], in_=xq[ti * 128:(ti + 1) * 128, :]), writes=[("xt3", s)])
                    for n in range(4):
                        for h in range(NH):
                            tr.op("pe", lambda: pe.matmul(pY[:, n, :], lhsT=oT[:, s, h, :], rhs=wo[:, h, n * 512:(n + 1) * 512],
                                                           start=(h == 0), stop=(h == NH - 1)),
                                  reads=[("oT", s), "wo"], writes=[("pTr", n)])
                    tr.op("dve", lambda: dve.tensor_tensor(out=zt[:], in0=R.pTr[:].rearrange("p a b c -> p (a b c)"), in1=g1b[:], op=ALU.mult),
                          reads=[("pTr", n) for n in range(4)] + [g1b.name], writes=["zt"])
                    tr.op("dve", lambda: dve.scalar_tensor_tensor(out=zt[:], in0=xt[:, s, :], scalar=ALPHA, in1=zt[:], op0=ALU.mult, op1=ALU.add),
                          reads=[("xt3", s), "zt"], writes=["zt"])
                    ln_router(R, ti, zt, "zt", x1s)
                tr.flush()
            with ExitStack() as st3:
                dispatch(R, st3)
                if stop_after >= 4:
                    moe_experts(0, R)
                if stop_after >= 5:
                    combine(R, 0, NT, x1s, final=False)

    def layer1():
        with ExitStack() as stack:
            with ExitStack() as st2:
                R = alloc_router(stack, st2, 1, 16)
                e = st2.enter_context
                wp = e(nc.sbuf_tensor("p6_wp", [128, 4, 4, 512], BF16))
                Bm = e(nc.sbuf_tensor("p6_B", [128, 4, 7, 128], BF16))
                g1b = e(nc.sbuf_tensor("p6_g1", [128, D], F32))
                uu = e(nc.sbuf_tensor("p6_u", [128, NT, D], BF16))
                xt = e(nc.sbuf_tensor("p6_x", [128, 2, D], F32))
                dT = e(nc.sbuf_tensor("p6_dT", [128, 16, 128], BF16))
                zt = e(nc.sbuf_tensor("p6_z", [128, D], F32))
                psb, s1p, s1h = zt, R.x1, R.hh
                pD_ = R.pTr[:].rearrange("p a b c -> p (a b) c")
                tr.dma("pool", lambda: pool.dma_start(out=wp[:], in_=w_pool.rearrange("g (k p) n -> p g k n", p=128)), writes=["wp"])
                tr.dma("pool", lambda: pool.dma_start(out=Bm[:].rearrange("p a b c -> p (a b c)"), in_=poolB), writes=["Bm"])
                tr.dma("sp", lambda: sp.dma_start(out=psb[:], in_=pool_scale.partition_broadcast(128)), writes=[psb.name])
                mod_bcast(g1b, 1, 0, 2)
                mod_bcast(s1p, 1, 0, 1)
                mod_bcast(s1h, 1, 0, 0)
                tr.op("pool", lambda: pool.tensor_scalar(out=s1p[:], in0=s1p[:], scalar1=1.0, scalar2=None, op0=ALU.add),
                      reads=[s1p.name], writes=[s1p.name])
                tr.op("pool", lambda: pool.tensor_tensor(out=g1b[:], in0=g1b[:], in1=psb[:], op=ALU.mult),
                      reads=[g1b.name, psb.name], writes=[g1b.name])
                for ti in range(NT):
                    s = ti % 2
                    tr.dma("sp", lambda: sp.dma_start(out=xt[:, s, :], in_=x2s[ti * 128:(ti + 1) * 128, :]), writes=[("xt6", s)])
                    tr.op("dve", lambda: dve.tensor_tensor(out=xt[:, s, :], in0=xt[:, s, :], in1=s1p[:], op=ALU.mult),
                          reads=[("xt6", s), s1p.name], writes=[("xt6", s)])
                    tr.op("pool", lambda: pool.tensor_tensor(out=uu[:, ti, :], in0=xt[:, s, :], in1=s1h[:], op=ALU.add),
                          reads=[("xt6", s), s1h.name], writes=[("uu", ti)])
                for ti in range(16):
                    s = ti % 2
                    srcs = [(ti - 1, 0) if ti > 0 else (16, 3), (ti, 5 if ti == 0 else (6 if ti == 15 else 1)),
                            (ti + 1, 2) if ti < 15 else (16, 4)]
                    for g in range(4):
                        for fc in range(4):
                            f = g * 4 + fc
                            for si, (st_, kind) in enumerate(srcs):
                                tr.op("pe", lambda: pe.matmul(pD_[:, f, :], lhsT=uu[:, st_, f * 128:(f + 1) * 128], rhs=Bm[:, g, kind, :],
                                                               start=(si == 0), stop=(si == 2)),
                                      reads=[("uu", st_), "Bm"], writes=[("pTr", f // 4)])
                    for g in range(4):
                        if g % 2 == 0:
                            tr.op("act", lambda: act.copy(out=dT[:, g * 4:(g + 1) * 4, :], in_=pD_[:, g * 4:(g + 1) * 4, :]),
                                  reads=[("pTr", g)], writes=[("dT", g)])
                        else:
                            tr.op("dve", lambda: dve.tensor_copy(out=dT[:, g * 4:(g + 1) * 4, :], in_=pD_[:, g * 4:(g + 1) * 4, :]),
                                  reads=[("pTr", g)], writes=[("dT", g)])
                    pY = R.pTr[:].rearrange("p a b c -> p a (b c)")
                    for g in range(4):
                        for fc in range(4):
                            tr.op("pe", lambda: pe.matmul(pY[:, g, :], lhsT=dT[:, g * 4 + fc, :], rhs=wp[:, g, fc, :], start=(fc == 0), stop=(fc == 3)),
                                  reads=[("dT", g), "wp"], writes=[("pTr", g)])
                    tr.dma("sp", lambda: sp.dma_start(out=xt[:, s, :], in_=x2s[ti * 128:(ti + 1) * 128, :]), writes=[("xt6", s)])
                    tr.op("dve", lambda: dve.tensor_tensor(out=zt[:], in0=R.pTr[:].rearrange("p a b c -> p (a b c)"), in1=g1b[:], op=ALU.mult),
                          reads=[("pTr", g) for g in range(4)] + [g1b.name], writes=["zt"])
                    tr.op("dve", lambda: dve.scalar_tensor_tensor(out=zt[:], in0=xt[:, s, :], scalar=ALPHA, in1=zt[:], op0=ALU.mult, op1=ALU.add),
                          reads=[("xt6", s), "zt"], writes=["zt"])
                    ln_router(R, ti, zt, "zt", x1s)
                tr.flush()
            with ExitStack() as st3:
                dispatch(R, st3)
                moe_experts(1, R)
                combine(R, 1, 16, x1s, final=True)

    if MOEONLY:
        sei_in = din("sei_in", [1, 2 * NE], I32)
        stt_in = din("stt_in", [1, NE], F32)
        with ExitStack() as _st:
            R = RouterState()
            R.sei = _st.enter_context(nc.sbuf_tensor("mo_sei", [128, 2 * NE], I32))
            R.stt = _st.enter_context(nc.sbuf_tensor("mo_stt", [128, NE], F32))
            tr.dma("sp", lambda: sp.dma_start(out=R.sei[:], in_=sei_in.partition_broadcast(128)), writes=["sei"])
            tr.dma("sp", lambda: sp.dma_start(out=R.stt[:], in_=stt_in.partition_broadcast(128)), writes=["stt"])
            tr.flush()
            moe_experts(0, R)
        tr.flush()
        return nc
    phase0()
    if stop_after >= 1:
        phase1()
    if stop_after >= 2:
        phase2()
    if stop_after >= 3:
        layer0_post()
    if stop_after >= 6:
        layer1()
    tr.flush()
    return nc


def _rope_tables(pos):
    pos = np.asarray(pos)
    row = (pos // 64).astype(np.float32)
    col = (pos % 64).astype(np.float32)
    inv = (np.float32(10000.0) ** (-np.arange(32, dtype=np.float32) / np.float32(32))).astype(np.float32)
    ar = row[None, :] * inv[:, None]
    ac = col[None, :] * inv[:, None]
    ang = np.concatenate([ar, ar, ac, ac], axis=0).astype(np.float32)
    cos = np.cos(ang).astype(np.float32)
    sin = np.sin(ang).astype(np.float32)
    sgn = np.ones((128, 1), np.float32)
    sgn[0:32] = -1
    sgn[64:96] = -1
    return cos, (sin * sgn).astype(np.float32)


def _pool_tables(qr):
    B = np.zeros((128, 4, 7, 128), np.float32)
    L = SEQ
    base = qr * OWN

    def fill(kind, g, out_tile, src_of):
        w = WINS[g]
        for oc in range(128):
            t = base + out_tile * 128 + oc
            lo = max(t - w // 2, 0)
            hi = min(t + w // 2, L)
            cntv = hi - lo
            for tp in range(lo, hi):
                r = src_of(tp)
                if r is not None:
                    B[r, g, kind, oc] += 1.0 / cntv
            r = src_of(t)
            if r is not None:
                B[r, g, kind, oc] -= 1.0

    for g in range(4):
        mid = 7
        fill(0, g, mid, lambda tp: (tp - (base + (mid - 1) * 128)) if base + (mid - 1) * 128 <= tp < base + mid * 128 else None)
        fill(1, g, mid, lambda tp: (tp - (base + mid * 128)) if base + mid * 128 <= tp < base + (mid + 1) * 128 else None)
        fill(2, g, mid, lambda tp: (tp - (base + (mid + 1) * 128)) if base + (mid + 1) * 128 <= tp < base + (mid + 2) * 128 else None)
        fill(3, g, 0, lambda tp: (tp - (base - 8)) if base - 8 <= tp < base else None)
        fill(4, g, 15, lambda tp: (8 + tp - (base + OWN)) if base + OWN <= tp < base + OWN + 8 else None)
        fill(5, g, 0, lambda tp: (tp - base) if base <= tp < base + 128 else None)
        fill(6, g, 15, lambda tp: (tp - (base + 15 * 128)) if base + 15 * 128 <= tp < base + 16 * 128 else None)
    return B.reshape(128, 4 * 7 * 128)


def make_in_maps(inputs, cores=range(8)):
    x = np.asarray(inputs["x"], np.float32)
    c = np.asarray(inputs["c"], np.float32)
    ctx = np.asarray(inputs["ctx"], np.float32)
    c_ctx = np.asarray(inputs["c_ctx"], np.float32)
    ident = np.eye(128, dtype=np.float32)
    perm = np.zeros((128, 128), np.float32)
    for m in range(128):
        perm[m + 32 if (m % 64) < 32 else m - 32, m] = 1.0
    triu = np.triu(np.ones((128, 128), np.float32), k=1)
    cosK, sinK = _rope_tables(np.arange(SEQ))
    shared = {
        "w_mod": np.asarray(inputs["w_mod"], np.float32), "b_mod": np.asarray(inputs["b_mod"], np.float32),
        "ln_g": np.asarray(inputs["ln_g"], np.float32), "ln_b": np.asarray(inputs["ln_b"], np.float32),
        "w_qkv": np.asarray(inputs["w_qkv"], np.float32)[0],
        "q_gain": np.asarray(inputs["q_gain"], np.float32).reshape(128, 1),
        "k_gain": np.asarray(inputs["k_gain"], np.float32).reshape(128, 1),
        "w_o": np.asarray(inputs["w_o"], np.float32)[0],
        "w_pool": np.asarray(inputs["w_pool"], np.float32)[0],
        "pool_scale": np.asarray(inputs["pool_scale"], np.float32).reshape(1, D),
        "w_router": np.asarray(inputs["w_router"], np.float32),
        "router_bias": np.asarray(inputs["router_bias"], np.float32).reshape(1, NE),
        "w_gate": np.asarray(inputs["w_gate"], np.float32), "w_up": np.asarray(inputs["w_up"], np.float32),
        "w_down": np.asarray(inputs["w_down"], np.float32),
        "iotap": np.arange(128, dtype=np.float32).reshape(128, 1), "ident": ident, "perm": perm, "triu": triu, "cosK": cosK, "sinK": sinK,
    }
    maps = []
    for core in cores:
        b, qr = core // 4, core % 4
        base = qr * OWN
        loc = list(range(base, base + OWN))
        halo = [min(max(t, 0), SEQ - 1) for t in list(range(base - 8, base)) + list(range(base + OWN, base + OWN + 8))]
        loc = loc + halo + [base] * (NTOK - OWN - 16)
        loc = np.asarray(loc)
        cpad = np.zeros((128, 16, 33), np.float32)
        cpad[:, :, 0] = c[b].reshape(16, 128).T
        cpad[:, :, 32] = c_ctx.reshape(16, 128).T
        cosQ, sinQ = _rope_tables(loc)
        m = dict(shared)
        m.update({
            "keysrc": np.concatenate([ctx[b], x[b]], axis=0),
            "xq": x[b][loc],
            "cpad": cpad.reshape(128, 16 * 33),
            "cosQ": cosQ, "sinQ": sinQ,
            "poolB": _pool_tables(qr),
        })
        maps.append(m)
    return maps


def kernel(**inputs):
    nc = build()
    maps = make_in_maps(inputs)
    res = run_bass_kernel_spmd(nc, maps, core_ids=list(range(8)))
    outs = [np.asarray(r["out"], np.float32) for r in res.results]
    full = np.zeros((2, SEQ, D), np.float32)
    for core in range(8):
        b, qr = core // 4, core % 4
        full[b, qr * OWN:(qr + 1) * OWN] = outs[core]
    return full
```

```python
import os
import numpy as np
from contextlib import ExitStack
import concourse.bass as bass
import concourse.mybir as mybir
from concourse.bass_utils import run_bass_kernel_spmd

F32 = mybir.dt.float32
BF16 = mybir.dt.bfloat16
I32 = mybir.dt.int32
ALU = mybir.AluOpType
AF = mybir.ActivationFunctionType
AX = mybir.AxisListType

D = 2048
SEQ = 8192
CTX = 256
NKEY = SEQ + CTX
NKT = NKEY // 128
OWN = 2048
NT = 17
NTOK = NT * 128
NH = 16
NKV = 4
HD = 128
NE = 32
DE = 1024
NBLK = 68
ALPHA = float((2 * 2) ** 0.25)
LN_EPS = 1e-6
RMS_EPS = 1e-6
ATTN_SCALE = float(HD ** -0.5)
WINS = (2, 4, 8, 16)


class TR:
    def __init__(self, nc, ndq=24):
        self.nc = nc
        self.eng = {"pe": nc.tensor, "act": nc.scalar, "dve": nc.vector, "pool": nc.gpsimd, "sp": nc.sync}
        self.csem = {e: nc.alloc_semaphore(name=f"cs_{e}") for e in ("pe", "act", "dve", "pool")}
        self.cnt = {e: 0 for e in self.csem}
        self.dsem = {q: [nc.alloc_semaphore(name=f"ds_{q}{i}") for i in range(ndq)] for q in ("sp", "pool")}
        self.dval = {q: [0] * ndq for q in ("sp", "pool")}
        self.dpos = {"sp": 0, "pool": 0}
        self.seen = {e: {} for e in self.eng}
        self.lastw = {}
        self.readers = {}

    def _sem(self, tk):
        return self.csem[tk[1]] if tk[0] == "c" else self.dsem[tk[1]][tk[2]]

    def wait(self, E, tok):
        tk, val = tok
        if tk[0] == "c" and tk[1] == E and E == "pe":
            return
        if self.seen[E].get(tk, 0) >= val:
            return
        self.eng[E].wait_ge(self._sem(tk), val)
        self.seen[E][tk] = val

    def _deps(self, E, reads, writes):
        for k in reads:
            t = self.lastw.get(k)
            if t:
                self.wait(E, t)
        for k in writes:
            t = self.lastw.get(k)
            if t:
                self.wait(E, t)
            for tk, val in self.readers.get(k, {}).items():
                self.wait(E, (tk, val))

    def _rec(self, tok, reads, writes):
        tk, val = tok
        for k in reads:
            self.readers.setdefault(k, {})[tk] = val
        for k in writes:
            self.lastw[k] = tok
            self.readers[k] = {}

    def op(self, E, fn, reads=(), writes=()):
        self._deps(E, reads, writes)
        inst = fn()
        self.cnt[E] += 1
        inst.then_inc(self.csem[E], 1)
        self._rec((("c", E), self.cnt[E]), reads, writes)

    def dma(self, Q, fn, reads=(), writes=()):
        self._deps(Q, reads, writes)
        i = self.dpos[Q] % len(self.dsem[Q])
        self.dpos[Q] += 1
        tk = ("d", Q, i)
        if self.dval[Q][i] > 0:
            self.wait(Q, (tk, self.dval[Q][i]))
        inst = fn()
        self.dval[Q][i] += 16
        inst.then_inc(self.dsem[Q][i], 16)
        self._rec((tk, self.dval[Q][i]), reads, writes)

    def flush(self):
        for Q in ("sp", "pool"):
            for i, v in enumerate(self.dval[Q]):
                if v > 0:
                    self.wait("sp", (("d", Q, i), v))
        for e in self.csem:
            if self.cnt[e] > 0:
                self.wait("sp", (("c", e), self.cnt[e]))
        self.nc.all_engine_barrier()
        for E in self.seen:
            for Q in ("sp", "pool"):
                for i, v in enumerate(self.dval[Q]):
                    self.seen[E][("d", Q, i)] = v
            for e in self.csem:
                self.seen[E][("c", e)] = self.cnt[e]
        self.lastw = {}
        self.readers = {}


def build(stop_after=99, debug=False):
    nc = bass.Bass("TRN2", target_bir_lowering=False)
    tr = TR(nc)
    pe, act, dve, pool, sp = nc.tensor, nc.scalar, nc.vector, nc.gpsimd, nc.sync

    def din(name, shape, dt=F32):
        if os.environ.get("MOEONLY") and name not in ("xs", "w_gate", "w_up", "w_down", "sei_in", "stt_in", "ident", "iotap"):
            return nc.dram_tensor(name, list(shape), dt, kind="Internal").ap()
        return nc.dram_tensor(name, list(shape), dt, kind="ExternalInput").ap()

    def dscr(name, shape, dt):
        kind = "ExternalOutput" if (debug and name in os.environ.get("DBG", "").split(",")) else "Internal"
        return nc.dram_tensor(name, list(shape), dt, kind=kind).ap()

    keysrc = din("keysrc", [NKEY, D])
    xq = din("xq", [NTOK, D])
    cpad = din("cpad", [128, 16 * 33])
    w_mod = din("w_mod", [2, D, 6 * D])
    b_mod = din("b_mod", [2, 6 * D])
    ln_g = din("ln_g", [2, 2, D])
    ln_b = din("ln_b", [2, 2, D])
    w_qkv = din("w_qkv", [D, 3072])
    q_gain = din("q_gain", [128, 1])
    k_gain = din("k_gain", [128, 1])
    w_o = din("w_o", [D, D])
    w_pool = din("w_pool", [4, 512, 512])
    pool_scale = din("pool_scale", [1, D])
    w_router = din("w_router", [D, NE])
    router_bias = din("router_bias", [1, NE])
    MOEONLY = bool(os.environ.get("MOEONLY"))
    NEX = int(os.environ.get("MOE_NE", str(NE)))
    if stop_after >= 4:
        w_gate = din("w_gate", [2, NEX, D, DE])
        w_up = din("w_up", [2, NEX, D, DE])
        w_down = din("w_down", [2, NEX, DE, D])
    ident_in = din("ident", [128, 128])
    perm_in = din("perm", [128, 128])
    triu_in = din("triu", [128, 128])
    cosK = din("cosK", [128, SEQ])
    sinK = din("sinK", [128, SEQ])
    cosQ = din("cosQ", [128, NTOK])
    sinQ = din("sinQ", [128, NTOK])
    poolB = din("poolB", [128, 4 * 7 * 128])
    iotap = din("iotap", [128, 1])
    out = nc.dram_tensor("out", [OWN, D], F32, kind="ExternalOutput").ap()

    modrow = dscr("modrow", [2, 2, 6 * D], F32)
    KT = dscr("KT", [NKV, 128, NKEY], BF16)
    Vt = dscr("Vt", [NKEY, 512], BF16)
    QT = dscr("QT", [NH, 128, NTOK], BF16)
    OTs = dscr("OTs", [NH, 128, NTOK], BF16)
    x1s = dscr("x1s", [NTOK, D], F32)
    x2s = dscr("x2s", [NTOK, D], F32)
    hs = dscr("hs", [NTOK, D], BF16)
    xs = din("xs", [NBLK * 128, D], BF16) if MOEONLY else dscr("xs", [NBLK * 128, D], BF16)
    ys0 = dscr("ys0", [NBLK * 128, D], F32)
    ys1 = dscr("ys1", [NBLK * 128, D], F32)
    ys = [ys0, ys1]

    CH = 512

    def phase0():
        with ExitStack() as _st:
            cT = _st.enter_context(nc.sbuf_tensor("p0_c", [128, 16 * 33], F32))
            sT = _st.enter_context(nc.sbuf_tensor("p0_s", [128, 16 * 33], F32))
            wt = _st.enter_context(nc.sbuf_tensor("p0_w", [128, 2, 16, CH], F32))
            bt = _st.enter_context(nc.sbuf_tensor("p0_b", [33, 2, CH], F32))
            ot = _st.enter_context(nc.sbuf_tensor("p0_o", [33, 2, CH], F32))
            ps = _st.enter_context(nc.psum_tensor("p0_ps", [33, 2, CH], F32))
            tr.dma("sp", lambda: sp.dma_start(out=cT[:], in_=cpad), writes=["cT"])
            tr.op("act", lambda: act.activation(out=sT[:], in_=cT[:], func=AF.Silu), reads=["cT"], writes=["sT"])
            it = 0
            for i in range(2):
                wsrc = w_mod[i].rearrange("(k p) n -> p k n", p=128)
                for n in range(24):
                    s = it % 2
                    it += 1
                    tr.dma("sp", lambda: sp.dma_start(out=wt[:, s], in_=wsrc[:, :, n * CH:(n + 1) * CH]),
                           writes=[("wt", s)])
                    tr.dma("sp", lambda: sp.dma_start(out=bt[0:1, s, :], in_=b_mod[i:i + 1, n * CH:(n + 1) * CH]),
                           writes=[("bt0", s)])
                    tr.dma("sp", lambda: sp.dma_start(out=bt[32:33, s, :], in_=b_mod[i:i + 1, n * CH:(n + 1) * CH]),
                           writes=[("bt1", s)])
                    for k in range(16):
                        tr.op("pe", lambda: pe.matmul(ps[:, s, :], lhsT=sT[:, k * 33:(k + 1) * 33], rhs=wt[:, s, k, :],
                                                       start=(k == 0), stop=(k == 15)),
                              reads=["sT", ("wt", s)], writes=[("ps0", s)])
                    tr.op("dve", lambda: dve.tensor_tensor(out=ot[0:1, s, :], in0=ps[0:1, s, :], in1=bt[0:1, s, :], op=ALU.add),
                          reads=[("ps0", s), ("bt0", s)], writes=[("ot0", s)])
                    tr.op("dve", lambda: dve.tensor_tensor(out=ot[32:33, s, :], in0=ps[32:33, s, :], in1=bt[32:33, s, :], op=ALU.add),
                          reads=[("ps0", s), ("bt1", s)], writes=[("ot1", s)])
                    tr.dma("sp", lambda: sp.dma_start(out=modrow[i, 0:1, n * CH:(n + 1) * CH], in_=ot[0:1, s, :]),
                           reads=[("ot0", s)], writes=["modrow"])
                    tr.dma("sp", lambda: sp.dma_start(out=modrow[i, 1:2, n * CH:(n + 1) * CH], in_=ot[32:33, s, :]),
                           reads=[("ot1", s)], writes=["modrow"])
            tr.flush()

    def mod_bcast(t, layer, which, idx):
        tr.dma("sp", lambda: sp.dma_start(out=t[:], in_=modrow[layer, which:which + 1, idx * D:(idx + 1) * D].partition_broadcast(128)),
               reads=["modrow"], writes=[t.name])

    def mod_col(t, layer, which, idx):
        with nc.allow_non_contiguous_dma("tiny column load"):
            tr.dma("sp", lambda: sp.dma_start(out=t[:], in_=modrow[layer, which, idx * D:(idx + 1) * D].rearrange("(k p) -> p k", p=128)),
                   reads=["modrow"], writes=[t.name])

    def phase1():
        with ExitStack() as _st:
            wkv = _st.enter_context(nc.sbuf_tensor("p1_wkv", [128, 16, 1024], BF16))
            wq = _st.enter_context(nc.sbuf_tensor("p1_wq", [128, 16, 2048], BF16))
            ident = _st.enter_context(nc.sbuf_tensor("p1_id", [128, 128], F32))
            perm = _st.enter_context(nc.sbuf_tensor("p1_pm", [128, 128], BF16))
            ones = _st.enter_context(nc.sbuf_tensor("p1_on", [128, 128], BF16))
            epst = _st.enter_context(nc.sbuf_tensor("p1_eps", [128, 1], F32))
            gq = _st.enter_context(nc.sbuf_tensor("p1_gq", [128, 1], F32))
            gk = _st.enter_context(nc.sbuf_tensor("p1_gk", [128, 1], F32))
            shl = _st.enter_context(nc.sbuf_tensor("p1_shl", [128, 16], F32))
            scl = _st.enter_context(nc.sbuf_tensor("p1_scl", [128, 16], F32))
            shc = _st.enter_context(nc.sbuf_tensor("p1_shc", [128, 16], F32))
            scc = _st.enter_context(nc.sbuf_tensor("p1_scc", [128, 16], F32))
            xt = _st.enter_context(nc.sbuf_tensor("p1_x", [128, 2, D], F32))
            uT = _st.enter_context(nc.sbuf_tensor("p1_u", [128, 16, CH], BF16))
            cost = _st.enter_context(nc.sbuf_tensor("p1_cos", [128, 2, CH], F32))
            sint = _st.enter_context(nc.sbuf_tensor("p1_sin", [128, 2, CH], F32))
            sq = _st.enter_context(nc.sbuf_tensor("p1_sq", [128, 2, CH], BF16))
            kg = _st.enter_context(nc.sbuf_tensor("p1_kg", [128, 2, CH], BF16))
            rs = _st.enter_context(nc.sbuf_tensor("p1_rs", [128, 2, CH], F32))
            t1 = _st.enter_context(nc.sbuf_tensor("p1_t1", [128, 2, CH], F32))
            t2 = _st.enter_context(nc.sbuf_tensor("p1_t2", [128, 2, CH], F32))
            ko = _st.enter_context(nc.sbuf_tensor("p1_ko", [128, 2, CH], BF16))
            vo = _st.enter_context(nc.sbuf_tensor("p1_vo", [128, 2, 512], BF16))
            pT = _st.enter_context(nc.psum_tensor("p1_pT", [128, 2, 4, 128], F32))
            pA = _st.enter_context(nc.psum_tensor("p1_pA", [128, 2, CH], F32))
            pB = _st.enter_context(nc.psum_tensor("p1_pB", [128, 2, CH], F32))
            pC = _st.enter_context(nc.psum_tensor("p1_pC", [128, 2, CH], F32))
            wsrc = w_qkv.rearrange("(k p) n -> p k n", p=128)
            for k4 in range(4):
                tr.dma("pool", lambda: pool.dma_start(out=wkv[:, k4 * 4:(k4 + 1) * 4, :], in_=wsrc[:, k4 * 4:(k4 + 1) * 4, 2048:3072]),
                       writes=[wkv.name])
            for k4 in range(8):
                for hh_ in range(2):
                    tr.dma("pool", lambda: pool.dma_start(out=wq[:, k4 * 2:(k4 + 1) * 2, hh_ * 1024:(hh_ + 1) * 1024],
                                                          in_=wsrc[:, k4 * 2:(k4 + 1) * 2, hh_ * 1024:(hh_ + 1) * 1024]),
                           writes=[wq.name])
            tr.dma("sp", lambda: sp.dma_start(out=ident[:], in_=ident_in), writes=["ident"])
            tr.dma("pool", lambda: pool.dma_start(out=perm[:], in_=perm_in), writes=["perm"])
            tr.dma("sp", lambda: sp.dma_start(out=gq[:], in_=q_gain), writes=[gq.name])
            tr.dma("sp", lambda: sp.dma_start(out=gk[:], in_=k_gain), writes=[gk.name])
            tr.op("dve", lambda: dve.memset(ones[:], 1.0), writes=["ones"])
            tr.op("dve", lambda: dve.memset(epst[:], RMS_EPS), writes=["eps"])
            mod_col(shl, 0, 0, 0)
            mod_col(scl, 0, 0, 1)
            mod_col(shc, 0, 1, 0)
            mod_col(scc, 0, 1, 1)
            tr.op("dve", lambda: dve.tensor_scalar(out=scl[:], in0=scl[:], scalar1=1.0, scalar2=None, op0=ALU.add),
                  reads=[scl.name], writes=[scl.name])
            tr.op("dve", lambda: dve.tensor_scalar(out=scc[:], in0=scc[:], scalar1=1.0, scalar2=None, op0=ALU.add),
                  reads=[scc.name], writes=[scc.name])

            cnt = {"x": 0, "q": 0}

            def load_uT(src, row0, ntile, sh, sc):
                for t in range(ntile):
                    s = cnt["x"] % 2
                    cnt["x"] += 1
                    tr.dma("sp", lambda: sp.dma_start(out=xt[:, s, :], in_=src[row0 + t * 128: row0 + (t + 1) * 128, :]),
                           writes=[("xt", s)])
                    for g in range(4):
                        b = g % 2
                        for kk in range(4):
                            k = g * 4 + kk
                            tr.op("pe", lambda: pe.transpose(pT[:, b, kk, :], xt[:, s, k * 128:(k + 1) * 128], ident[:]),
                                  reads=[("xt", s), "ident"], writes=[("pT", b)])
                        for kk in range(4):
                            k = g * 4 + kk
                            if os.environ.get("NOMOD"):
                                tr.op("dve", lambda: dve.tensor_copy(out=uT[:, k, t * 128:(t + 1) * 128], in_=pT[:, b, kk, :]),
                                      reads=[("pT", b)], writes=["uT"])
                            elif kk % 2 == 0:
                                tr.op("act", lambda: act.activation(out=uT[:, k, t * 128:(t + 1) * 128], in_=pT[:, b, kk, :],
                                                                    func=AF.Identity, scale=sc[:, k:k + 1], bias=sh[:, k:k + 1]),
                                      reads=[("pT", b), sc.name, sh.name], writes=["uT"])
                            else:
                                tr.op("dve", lambda: dve.tensor_scalar(out=uT[:, k, t * 128:(t + 1) * 128], in0=pT[:, b, kk, :],
                                                                       scalar1=sc[:, k:k + 1], scalar2=sh[:, k:k + 1],
                                                                       op0=ALU.mult, op1=ALU.add),
                                      reads=[("pT", b), sc.name, sh.name], writes=["uT"])

            def proj_T(w, col0, n, gain, cos_src, sin_src, pos0, dst):
                pc = int(os.environ.get("PCUT", "9"))
                s = cnt["q"] % 2
                cnt["q"] += 1
                for k in range(16):
                    tr.op("pe", lambda: pe.matmul(pA[:, s, :n], lhsT=w[:, k, col0:col0 + 128], rhs=uT[:, k, :n],
                                                   start=(k == 0), stop=(k == 15)),
                          reads=["uT", w.name], writes=[("pA", s)])
                if pc < 2:
                    return
                tr.op("act", lambda: act.activation(out=sq[:, s, :n], in_=pA[:, s, :n], func=AF.Square),
                      reads=[("pA", s)], writes=[("sq", s)])
                if pc < 3:
                    return
                if os.environ.get("KGACT", "1") == "1":
                    tr.op("act", lambda: act.activation(out=kg[:, s, :n], in_=pA[:, s, :n], func=AF.Copy, scale=gain[:, 0:1]),
                          reads=[("pA", s), gain.name], writes=[("kg", s)])
                else:
                    tr.op("dve", lambda: dve.tensor_scalar(out=kg[:, s, :n], in0=pA[:, s, :n], scalar1=gain[:, 0:1], scalar2=None, op0=ALU.mult),
                          reads=[("pA", s), gain.name], writes=[("kg", s)])
                if pc < 4:
                    return
                tr.op("pe", lambda: pe.matmul(pB[:, s, :n], lhsT=ones[:], rhs=sq[:, s, :n], start=True, stop=True),
                      reads=[("sq", s), "ones"], writes=[("pB", s)])
                if pc < 5:
                    return
                tr.op("dve", lambda: dve.tensor_scalar(out=rs[:, s, :n], in0=pB[:, s, :n], scalar1=1.0 / HD, scalar2=RMS_EPS, op0=ALU.mult, op1=ALU.add),
                      reads=[("pB", s)], writes=[("rs", s)])
                tr.op("act", lambda: act.activation(out=rs[:, s, :n], in_=rs[:, s, :n], func=AF.Sqrt),
                      reads=[("rs", s)], writes=[("rs", s)])
                if pc < 6:
                    return
                tr.op("dve", lambda: dve.reciprocal(out=rs[:, s, :n], in_=rs[:, s, :n]), reads=[("rs", s)], writes=[("rs", s)])
                if pc < 7:
                    return
                if cos_src is not None:
                    tr.op("pe", lambda: pe.matmul(pC[:, s, :n], lhsT=perm[:], rhs=kg[:, s, :n], start=True, stop=True),
                          reads=[("kg", s), "perm"], writes=[("pC", s)])
                    tr.dma("sp", lambda: sp.dma_start(out=cost[:, s, :n], in_=cos_src[:, pos0:pos0 + n]), writes=[("cos", s)])
                    tr.dma("sp", lambda: sp.dma_start(out=sint[:, s, :n], in_=sin_src[:, pos0:pos0 + n]), writes=[("sin", s)])
                    tr.op("dve", lambda: dve.tensor_tensor(out=t1[:, s, :n], in0=kg[:, s, :n], in1=cost[:, s, :n], op=ALU.mult),
                          reads=[("kg", s), ("cos", s)], writes=[("t1", s)])
                    tr.op("act", lambda: act.copy(out=t2[:, s, :n], in_=pC[:, s, :n]), reads=[("pC", s)], writes=[("t2", s)])
                    tr.op("dve", lambda: dve.tensor_tensor(out=t2[:, s, :n], in0=t2[:, s, :n], in1=sint[:, s, :n], op=ALU.mult),
                          reads=[("t2", s), ("sin", s)], writes=[("t2", s)])
                    tr.op("dve", lambda: dve.tensor_tensor(out=t1[:, s, :n], in0=t1[:, s, :n], in1=t2[:, s, :n], op=ALU.add),
                          reads=[("t1", s), ("t2", s)], writes=[("t1", s)])
                    tr.op("dve", lambda: dve.tensor_tensor(out=ko[:, s, :n], in0=t1[:, s, :n], in1=rs[:, s, :n], op=ALU.mult),
                          reads=[("t1", s), ("rs", s)], writes=[("ko", s)])
                else:
                    tr.op("dve", lambda: dve.tensor_tensor(out=ko[:, s, :n], in0=kg[:, s, :n], in1=rs[:, s, :n], op=ALU.mult),
                          reads=[("kg", s), ("rs", s)], writes=[("ko", s)])
                if pc < 8:
                    return
                tr.dma("sp", lambda: sp.dma_start(out=dst, in_=ko[:, s, :n]), reads=[("ko", s)], writes=["KTQT"])

            cut = int(os.environ.get("K1CUT", "9"))
            chunks = [(0, 256, True)] + [(CTX + i * CH, CH, False) for i in range(SEQ // CH)]
            if cut == 0:
                chunks = []
            elif cut == 1:
                chunks = chunks[:1]
            elif cut == 2:
                chunks = chunks[:2]
            for (k0, n, is_ctx) in chunks:
                load_uT(keysrc, k0, n // 128, shc if is_ctx else shl, scc if is_ctx else scl)
                for j in range(NKV if not os.environ.get("SKIPK") else 0):
                    proj_T(wkv, j * 128, n, gk, None if is_ctx else cosK, None if is_ctx else sinK,
                           k0 - CTX, KT[j, :, k0:k0 + n])
                for t in range(n // 128 if not os.environ.get("SKIPV") else 0):
                    s = cnt["q"] % 2
                    cnt["q"] += 1
                    for k in range(16):
                        tr.op("pe", lambda: pe.matmul(pA[:, s, :], lhsT=uT[:, k, t * 128:(t + 1) * 128], rhs=wkv[:, k, 512:1024],
                                                       start=(k == 0), stop=(k == 15)),
                              reads=["uT", wkv.name], writes=[("pA", s)])
                    tr.op("act", lambda: act.copy(out=vo[:, s, :], in_=pA[:, s, :]), reads=[("pA", s)], writes=[("vo", s)])
                    tr.dma("sp", lambda: sp.dma_start(out=Vt[k0 + t * 128:k0 + (t + 1) * 128, :], in_=vo[:, s, :]),
                           reads=[("vo", s)], writes=["Vt"])
            for (q0, n) in ([(i * CH, CH) for i in range(4)] + [(2048, 128)] if cut >= 9 else []):
                load_uT(xq, q0, n // 128, shl, scl)
                for h in range(NH):
                    proj_T(wq, h * 128, n, gq, cosQ, sinQ, q0, QT[h, :, q0:q0 + n])
            tr.flush()

    def phase2():
        with ExitStack() as _st:
            Ks = _st.enter_context(nc.sbuf_tensor("p2_K", [128, NKV, NKEY], BF16))
            Vs = _st.enter_context(nc.sbuf_tensor("p2_V", [128, NKT, 512], BF16))
            ones = _st.enter_context(nc.sbuf_tensor("p2_on", [128, 128], BF16))
            qt = _st.enter_context(nc.sbuf_tensor("p2_q", [128, 2, CH], BF16))
            pt = _st.enter_context(nc.sbuf_tensor("p2_p", [128, 3, CH], BF16))
            rd = _st.enter_context(nc.sbuf_tensor("p2_rd", [128, 2, CH], F32))
            ot = _st.enter_context(nc.sbuf_tensor("p2_o", [128, 2, CH], BF16))
            pS = _st.enter_context(nc.psum_tensor("p2_pS", [128, 3, CH], F32))
            pO = _st.enter_context(nc.psum_tensor("p2_pO", [128, 2, CH], F32))
            pD = _st.enter_context(nc.psum_tensor("p2_pD", [128, 2, CH], F32))
            tr.op("dve", lambda: dve.memset(ones[:], 1.0), writes=["ones"])
            for j in range(NKV):
                for c in range(4):
                    c0, c1 = c * 2112, (c + 1) * 2112
                    tr.dma("sp", lambda: sp.dma_start(out=Ks[:, j, c0:c1], in_=KT[j, :, c0:c1]), writes=["Ks"])
            vsrc = Vt.rearrange("(t p) n -> p t n", p=128)
            for c in range(6):
                tr.dma("sp", lambda: sp.dma_start(out=Vs[:, c * 11:(c + 1) * 11, :], in_=vsrc[:, c * 11:(c + 1) * 11, :]), writes=["Vs"])
            it = 0
            sctr = 0
            for h in range(NH):
                j = h // 4
                for (q0, n) in [(i * CH, CH) for i in range(4)] + [(2048, 128)]:
                    b = it % 2
                    it += 1
                    tr.dma("sp", lambda: sp.dma_start(out=qt[:, b, :n], in_=QT[h, :, q0:q0 + n]), writes=[("qt", b)])

                    def smm(kt, s):
                        tr.op("pe", lambda: pe.matmul(pS[:, s, :n], lhsT=Ks[:, j, kt * 128:(kt + 1) * 128], rhs=qt[:, b, :n],
                                                       start=True, stop=True),
                              reads=["Ks", ("qt", b)], writes=[("pS", s)])
                        tr.op("act", lambda: act.activation(out=pt[:, s, :n], in_=pS[:, s, :n], func=AF.Exp, scale=ATTN_SCALE),
                              reads=[("pS", s)], writes=[("pt", s)])

                    base = sctr
                    smm(0, base % 3)
                    smm(1, (base + 1) % 3)
                    for kt in range(NKT):
                        s = (base + kt) % 3
                        if kt + 2 < NKT:
                            smm(kt + 2, (base + kt + 2) % 3)
                        tr.op("pe", lambda: pe.matmul(pO[:, b, :n], lhsT=Vs[:, kt, j * 128:(j + 1) * 128], rhs=pt[:, s, :n],
                                                       start=(kt == 0), stop=(kt == NKT - 1)),
                              reads=["Vs", ("pt", s)], writes=[("pO", b)])
                        tr.op("pe", lambda: pe.matmul(pD[:, b, :n], lhsT=ones[:], rhs=pt[:, s, :n],
                                                       start=(kt == 0), stop=(kt == NKT - 1)),
                              reads=["ones", ("pt", s)], writes=[("pD", b)])
                    sctr += NKT
                    tr.op("dve", lambda: dve.reciprocal(out=rd[:, b, :n], in_=pD[:, b, :n]), reads=[("pD", b)], writes=[("rd", b)])
                    tr.op("dve", lambda: dve.tensor_tensor(out=ot[:, b, :n], in0=pO[:, b, :n], in1=rd[:, b, :n], op=ALU.mult),
                          reads=[("pO", b), ("rd", b)], writes=[("ot", b)])
                    tr.dma("sp", lambda: sp.dma_start(out=OTs[h, :, q0:q0 + n], in_=ot[:, b, :n]), reads=[("ot", b)], writes=["OTs"])
            tr.flush()

    class RouterState:
        pass

    def alloc_router(stack, tstack, layer, ntile):
        R = RouterState()
        R.layer = layer
        R.ntile = ntile
        e = stack.enter_context
        te = tstack.enter_context
        R.epst = e(nc.sbuf_tensor(f"r{layer}_eps", [128, 1], F32))
        R.carry = e(nc.sbuf_tensor(f"r{layer}_cy", [128, NE], F32))
        R.A = e(nc.sbuf_tensor(f"r{layer}_A", [128, NT, NE], F32))
        R.pos = e(nc.sbuf_tensor(f"r{layer}_pos", [128, NT, NE], F32))
        R.gt = e(nc.sbuf_tensor(f"r{layer}_gt", [128, NT, NE], F32))
        R.st = e(nc.sbuf_tensor(f"r{layer}_st", [128, 4, 6], F32))
        R.mv = e(nc.sbuf_tensor(f"r{layer}_mv", [128, 2], F32))
        R.rstd = e(nc.sbuf_tensor(f"r{layer}_rstd", [128, 1], F32))
        R.lng = te(nc.sbuf_tensor(f"r{layer}_lng", [128, D], F32))
        R.lnb = te(nc.sbuf_tensor(f"r{layer}_lnb", [128, D], F32))
        R.scp = te(nc.sbuf_tensor(f"r{layer}_scp", [128, D], F32))
        R.shb = te(nc.sbuf_tensor(f"r{layer}_shb", [128, D], F32))
        R.wr = te(nc.sbuf_tensor(f"r{layer}_wr", [128, 16, NE], F32))
        R.rb = te(nc.sbuf_tensor(f"r{layer}_rb", [128, NE], F32))
        R.ident = te(nc.sbuf_tensor(f"r{layer}_id", [128, 128], F32))
        R.triu = te(nc.sbuf_tensor(f"r{layer}_tu", [128, 128], F32))
        R.onesf = te(nc.sbuf_tensor(f"r{layer}_on", [128, 128], F32))
        R.x1 = te(nc.sbuf_tensor(f"r{layer}_x1", [128, D], F32))
        R.hh = te(nc.sbuf_tensor(f"r{layer}_hh", [128, D], F32))
        R.hb = te(nc.sbuf_tensor(f"r{layer}_hb", [128, D], BF16))
        R.hT = te(nc.sbuf_tensor(f"r{layer}_hT", [128, 16, 128], F32))
        R.sm = te(nc.sbuf_tensor(f"r{layer}_sm", [128, 12, NE], F32))
        R.ext = te(nc.sbuf_tensor(f"r{layer}_ext", [128, 8, 8], F32))
        R.g8 = te(nc.sbuf_tensor(f"r{layer}_g8", [128, 8, 8], F32))
        R.s1 = te(nc.sbuf_tensor(f"r{layer}_s1", [128, 4], F32))
        R.pTr = te(nc.psum_tensor(f"r{layer}_pTr", [128, 4, 4, 128], F32))
        R.pSm = te(nc.psum_tensor(f"r{layer}_pSm", [128, 4, NE], F32))
        tr.dma("sp", lambda: sp.dma_start(out=R.lng[:], in_=ln_g[layer, 0:1, :].partition_broadcast(128)), writes=[R.lng.name])
        tr.dma("sp", lambda: sp.dma_start(out=R.lnb[:], in_=ln_b[layer, 0:1, :].partition_broadcast(128)), writes=[R.lnb.name])
        mod_bcast(R.scp, layer, 0, 4)
        mod_bcast(R.shb, layer, 0, 3)
        tr.op("pool", lambda: pool.tensor_scalar(out=R.scp[:], in0=R.scp[:], scalar1=1.0, scalar2=None, op0=ALU.add),
              reads=[R.scp.name], writes=[R.scp.name])
        tr.dma("sp", lambda: sp.dma_start(out=R.wr[:], in_=w_router.rearrange("(k p) e -> p k e", p=128)), writes=["wr"])
        tr.dma("sp", lambda: sp.dma_start(out=R.rb[:], in_=router_bias.partition_broadcast(128)), writes=["rb"])
        tr.dma("sp", lambda: sp.dma_start(out=R.ident[:], in_=ident_in), writes=["identr"])
        tr.dma("sp", lambda: sp.dma_start(out=R.triu[:], in_=triu_in), writes=["triu"])
        tr.op("dve", lambda: dve.memset(R.onesf[:], 1.0), writes=["onesf"])
        tr.op("dve", lambda: dve.memset(R.epst[:], LN_EPS), writes=["epsr"])
        tr.op("dve", lambda: dve.memset(R.carry[:], 0.0), writes=["carry"])
        return R

    def layer_norm(R, z, zkey, g, b, outt, outkey):
        for c in range(4):
            tr.op("dve", lambda: dve.bn_stats(out=R.st[:, c, :], in_=z[:, c * 512:(c + 1) * 512]), reads=[zkey], writes=["st"])
        tr.op("dve", lambda: dve.bn_aggr(out=R.mv[:], in_=R.st[:]), reads=["st"], writes=["mv"])
        tr.op("act", lambda: act.activation(out=R.rstd[:], in_=R.mv[:, 1:2], func=AF.Sqrt, scale=1.0, bias=R.epst[:, 0:1]),
              reads=["mv", "epsr"], writes=["rstd"])
        tr.op("dve", lambda: dve.reciprocal(out=R.rstd[:], in_=R.rstd[:]), reads=["rstd"], writes=["rstd"])
        tr.op("dve", lambda: dve.tensor_scalar(out=z[:], in0=z[:], scalar1=R.mv[:, 0:1], scalar2=R.rstd[:, 0:1],
                                               op0=ALU.subtract, op1=ALU.mult),
              reads=[zkey, "mv", "rstd"], writes=[zkey])
        tr.op("pool", lambda: pool.tensor_tensor(out=z[:], in0=z[:], in1=g[:], op=ALU.mult), reads=[zkey, g.name], writes=[zkey])
        tr.op("dve", lambda: dve.tensor_tensor(out=outt[:], in0=z[:], in1=b[:], op=ALU.add), reads=[zkey, b.name], writes=[outkey])

    def ln_router(R, ti, z, zkey, xdst):
        layer_norm(R, z, zkey, R.lng, R.lnb, R.x1, "x1")
        tr.dma("sp", lambda: sp.dma_start(out=xdst[ti * 128:(ti + 1) * 128, :], in_=R.x1[:]), reads=["x1"], writes=["xdst"])
        tr.op("pool", lambda: pool.tensor_tensor(out=R.hh[:], in0=R.x1[:], in1=R.scp[:], op=ALU.mult),
              reads=["x1", R.scp.name], writes=["hh"])
        tr.op("dve", lambda: dve.tensor_tensor(out=R.hh[:], in0=R.hh[:], in1=R.shb[:], op=ALU.add),
              reads=["hh", R.shb.name], writes=["hh"])
        tr.op("act", lambda: act.copy(out=R.hb[:], in_=R.hh[:]), reads=["hh"], writes=["hb"])
        tr.dma("sp", lambda: sp.dma_start(out=hs[ti * 128:(ti + 1) * 128, :], in_=R.hb[:]), reads=["hb"], writes=["hs"])
        for g in range(4):
            for kk in range(4):
                k = g * 4 + kk
                tr.op("pe", lambda: pe.transpose(R.pTr[:, g, kk, :], R.hh[:, k * 128:(k + 1) * 128], R.ident[:]),
                      reads=["hh", "identr"], writes=[("pTr", g)])
            if g % 2 == 0:
                tr.op("act", lambda: act.copy(out=R.hT[:, g * 4:(g + 1) * 4, :], in_=R.pTr[:, g, :, :]),
                      reads=[("pTr", g)], writes=["hT"])
            else:
                tr.op("dve", lambda: dve.tensor_copy(out=R.hT[:, g * 4:(g + 1) * 4, :], in_=R.pTr[:, g, :, :]),
                      reads=[("pTr", g)], writes=["hT"])
        for k in range(16):
            tr.op("pe", lambda: pe.matmul(R.pSm[:, 0, :], lhsT=R.hT[:, k, :], rhs=R.wr[:, k, :], start=(k == 0), stop=(k == 15)),
                  reads=["hT", "wr"], writes=["pLog"])
        sm = R.sm
        AFF, SEL, CNTR, SELM, GM, T0, T1 = (sm[:, i, :] for i in range(7))
        k_sm = "sm"
        tr.op("act", lambda: act.activation(out=AFF, in_=R.pSm[:, 0, :], func=AF.Sigmoid), reads=["pLog"], writes=[k_sm])
        tr.op("dve", lambda: dve.tensor_tensor(out=SEL, in0=AFF, in1=R.rb[:], op=ALU.add), reads=[k_sm, "rb"], writes=[k_sm])
        sel3 = sm[:, 1, :].rearrange("p (g j) -> p g j", j=4)
        tr.op("dve", lambda: dve.tensor_copy(out=R.ext[:, :, 0:4], in_=sel3), reads=[k_sm], writes=["ext"])
        tr.op("dve", lambda: dve.tensor_copy(out=R.ext[:, :, 4:8], in_=sel3), reads=[k_sm], writes=["ext"])
        cn3 = sm[:, 2, :].rearrange("p (g j) -> p g j", j=4)
        t03 = sm[:, 5, :].rearrange("p (g j) -> p g j", j=4)
        tr.op("dve", lambda: dve.tensor_tensor(out=cn3, in0=R.ext[:, :, 1:5], in1=sel3, op=ALU.is_gt), reads=["ext", k_sm], writes=[k_sm])
        for sft in (2, 3):
            tr.op("dve", lambda: dve.tensor_tensor(out=t03, in0=R.ext[:, :, sft:sft + 4], in1=sel3, op=ALU.is_gt),
                  reads=["ext", k_sm], writes=[k_sm])
            tr.op("dve", lambda: dve.tensor_tensor(out=cn3, in0=cn3, in1=t03, op=ALU.add), reads=[k_sm], writes=[k_sm])
        tr.op("dve", lambda: dve.tensor_scalar(out=SELM, in0=CNTR, scalar1=1.5, scalar2=None, op0=ALU.is_lt), reads=[k_sm], writes=[k_sm])
        tr.op("dve", lambda: dve.tensor_tensor(out=T0, in0=SELM, in1=SEL, op=ALU.mult), reads=[k_sm], writes=[k_sm])
        tr.op("dve", lambda: dve.tensor_reduce(out=R.g8[:, :, 0], in_=t03, axis=AX.X, op=ALU.add), reads=[k_sm], writes=["g8"])
        tr.op("dve", lambda: dve.tensor_reduce(out=R.s1[:, 0:1], in_=R.g8[:, :, 0], axis=AX.X, op=ALU.max), reads=["g8"], writes=["s1"])
        tr.op("dve", lambda: dve.tensor_scalar(out=R.g8[:, :, 1], in0=R.g8[:, :, 0], scalar1=R.s1[:, 0:1], scalar2=None, op0=ALU.is_ge),
              reads=["g8", "s1"], writes=["g8"])
        a3 = R.A[:, ti, :].rearrange("p (g j) -> p g j", j=4)
        sm3 = sm[:, 3, :].rearrange("p (g j) -> p g j", j=4)
        for jj in range(4):
            tr.op("dve", lambda: dve.tensor_tensor(out=a3[:, :, jj], in0=sm3[:, :, jj], in1=R.g8[:, :, 1], op=ALU.mult),
                  reads=[k_sm, "g8"], writes=[("A", ti)])
        tr.op("dve", lambda: dve.tensor_tensor(out=T1, in0=R.A[:, ti, :], in1=AFF, op=ALU.mult), reads=[("A", ti), k_sm], writes=[k_sm])
        tr.op("dve", lambda: dve.tensor_reduce(out=R.s1[:, 1:2], in_=T1, axis=AX.X, op=ALU.add), reads=[k_sm], writes=["s1b"])
        tr.op("dve", lambda: dve.reciprocal(out=R.s1[:, 2:3], in_=R.s1[:, 1:2]), reads=["s1b"], writes=["s1c"])
        tr.op("dve", lambda: dve.tensor_scalar(out=R.gt[:, ti, :], in0=T1, scalar1=R.s1[:, 2:3], scalar2=None, op0=ALU.mult),
              reads=[k_sm, "s1c"], writes=[("gt", ti)])
        tr.op("pe", lambda: pe.matmul(R.pSm[:, 1, :], lhsT=R.triu[:], rhs=R.A[:, ti, :], start=True, stop=True),
              reads=["triu", ("A", ti)], writes=["pPos"])
        tr.op("pe", lambda: pe.matmul(R.pSm[:, 2, :], lhsT=R.onesf[:], rhs=R.A[:, ti, :], start=True, stop=True),
              reads=["onesf", ("A", ti)], writes=["pCnt"])
        tr.op("dve", lambda: dve.tensor_tensor(out=R.pos[:, ti, :], in0=R.pSm[:, 1, :], in1=R.carry[:], op=ALU.add),
              reads=["pPos", "carry"], writes=[("pos", ti)])
        tr.op("dve", lambda: dve.tensor_tensor(out=R.carry[:], in0=R.pSm[:, 2, :], in1=R.carry[:], op=ALU.add),
              reads=["pCnt", "carry"], writes=["carry"])

    def dispatch(R, stack):
        e = stack.enter_context
        L = R.layer
        nb = e(nc.sbuf_tensor(f"d{L}_nb", [128, NE], F32))
        sc = e(nc.sbuf_tensor(f"d{L}_sc", [128, 2, NE], F32))
        stt = e(nc.sbuf_tensor(f"d{L}_st", [128, NE], F32))
        se = e(nc.sbuf_tensor(f"d{L}_se", [128, 2 * NE], F32))
        sei = e(nc.sbuf_tensor(f"d{L}_sei", [128, 2 * NE], I32))
        dd = e(nc.sbuf_tensor(f"d{L}_dd", [128, 3, NE], F32))
        R.slA = e(nc.sbuf_tensor(f"d{L}_slA", [128, NT], F32))
        R.slB = e(nc.sbuf_tensor(f"d{L}_slB", [128, NT], F32))
        R.gA = e(nc.sbuf_tensor(f"d{L}_gA", [128, NT], F32))
        R.gB = e(nc.sbuf_tensor(f"d{L}_gB", [128, NT], F32))
        R.slAi = e(nc.sbuf_tensor(f"d{L}_slAi", [128, NT], I32))
        R.slBi = e(nc.sbuf_tensor(f"d{L}_slBi", [128, NT], I32))
        hbuf = e(nc.sbuf_tensor(f"d{L}_hb", [128, 2, D], BF16))
        tr.op("dve", lambda: dve.memset(nb[:], 1.0), writes=["nb"])
        for jn in range(1, 18):
            tr.op("dve", lambda: dve.scalar_tensor_tensor(out=nb[:], in0=R.carry[:], scalar=float(128 * jn), in1=nb[:],
                                                         op0=ALU.is_gt, op1=ALU.add),
                  reads=["carry", "nb"], writes=["nb"])
        tr.op("dve", lambda: dve.tensor_copy(out=sc[:, 0, :], in_=nb[:]), reads=["nb"], writes=[("sc", 0)])
        cur = 0
        for dlt in (1, 2, 4, 8, 16):
            nxt = 1 - cur
            tr.op("dve", lambda: dve.tensor_copy(out=sc[:, nxt, 0:dlt], in_=sc[:, cur, 0:dlt]), reads=[("sc", cur)], writes=[("sc", nxt)])
            tr.op("dve", lambda: dve.tensor_tensor(out=sc[:, nxt, dlt:NE], in0=sc[:, cur, dlt:NE], in1=sc[:, cur, 0:NE - dlt], op=ALU.add),
                  reads=[("sc", cur)], writes=[("sc", nxt)])
            cur = nxt
        tr.op("dve", lambda: dve.tensor_tensor(out=stt[:], in0=sc[:, cur, :], in1=nb[:], op=ALU.subtract),
              reads=[("sc", cur), "nb"], writes=["stt"])
        tr.op("dve", lambda: dve.tensor_copy(out=se[:, 0:NE], in_=stt[:]), reads=["stt"], writes=["se"])
        tr.op("dve", lambda: dve.tensor_copy(out=se[:, NE:2 * NE], in_=sc[:, cur, :]), reads=[("sc", cur)], writes=["se"])
        tr.op("dve", lambda: dve.tensor_copy(out=sei[:], in_=se[:]), reads=["se"], writes=["sei"])
        for ti in range(R.ntile):
            DEST, VA, VB = dd[:, 0, :], dd[:, 1, :], dd[:, 2, :]
            tr.op("dve", lambda: dve.scalar_tensor_tensor(out=DEST, in0=stt[:], scalar=128.0, in1=R.pos[:, ti, :], op0=ALU.mult, op1=ALU.add),
                  reads=["stt", ("pos", ti)], writes=["dd"])
            tr.op("dve", lambda: dve.scalar_tensor_tensor(out=VA, in0=DEST, scalar=1.0, in1=R.A[:, ti, :], op0=ALU.add, op1=ALU.mult),
                  reads=["dd", ("A", ti)], writes=["dd"])
            tr.op("dve", lambda: dve.tensor_reduce(out=R.slA[:, ti:ti + 1], in_=VA, axis=AX.X, op=ALU.max), reads=["dd"], writes=["slA"])
            tr.op("dve", lambda: dve.tensor_scalar(out=VB, in0=R.A[:, ti, :], scalar1=-1.0e6, scalar2=1.0e6, op0=ALU.mult, op1=ALU.add),
                  reads=[("A", ti)], writes=["dd"])
            tr.op("dve", lambda: dve.tensor_tensor(out=VB, in0=VB, in1=DEST, op=ALU.add), reads=["dd"], writes=["dd"])
            tr.op("dve", lambda: dve.tensor_reduce(out=R.slB[:, ti:ti + 1], in_=VB, axis=AX.X, op=ALU.min), reads=["dd"], writes=["slB"])
            tr.op("dve", lambda: dve.tensor_scalar(out=VA, in0=VA, scalar1=R.slA[:, ti:ti + 1], scalar2=None, op0=ALU.is_ge),
                  reads=["dd", "slA"], writes=["dd"])
            tr.op("dve", lambda: dve.tensor_tensor(out=VA, in0=VA, in1=R.gt[:, ti, :], op=ALU.mult), reads=["dd", ("gt", ti)], writes=["dd"])
            tr.op("dve", lambda: dve.tensor_reduce(out=R.gA[:, ti:ti + 1], in_=VA, axis=AX.X, op=ALU.add), reads=["dd"], writes=["gA"])
        tr.op("dve", lambda: dve.tensor_scalar(out=R.slA[:], in0=R.slA[:], scalar1=-1.0, scalar2=None, op0=ALU.add), reads=["slA"], writes=["slA"])
        tr.op("dve", lambda: dve.tensor_scalar(out=R.gB[:], in0=R.gA[:], scalar1=-1.0, scalar2=1.0, op0=ALU.mult, op1=ALU.add),
              reads=["gA"], writes=["gB"])
        tr.op("dve", lambda: dve.tensor_copy(out=R.slAi[:], in_=R.slA[:]), reads=["slA"], writes=["slAi"])
        tr.op("dve", lambda: dve.tensor_copy(out=R.slBi[:], in_=R.slB[:]), reads=["slB"], writes=["slBi"])
        for ti in range(R.ntile):
            s = ti % 2
            tr.dma("sp", lambda: sp.dma_start(out=hbuf[:, s, :], in_=hs[ti * 128:(ti + 1) * 128, :]), reads=["hs"], writes=[("hbuf", s)])
            tr.dma("pool", lambda: pool.indirect_dma_start(out=xs[:, :], out_offset=bass.IndirectOffsetOnAxis(ap=R.slAi[:, ti:ti + 1], axis=0),
                                                           in_=hbuf[:, s, :], in_offset=None),
                   reads=[("hbuf", s), "slAi"], writes=["xs"])
            tr.dma("pool", lambda: pool.indirect_dma_start(out=xs[:, :], out_offset=bass.IndirectOffsetOnAxis(ap=R.slBi[:, ti:ti + 1], axis=0),
                                                           in_=hbuf[:, s, :], in_offset=None),
                   reads=[("hbuf", s), "slBi"], writes=["xs"])
        tr.flush()
        R.sei = sei
        R.stt = stt
        return None

    def moe_experts(layer, R):
        with ExitStack() as _st:
            wg = _st.enter_context(nc.sbuf_tensor(f"m{layer}_wg", [128, 3, 16, 512], BF16))
            wu = _st.enter_context(nc.sbuf_tensor(f"m{layer}_wu", [128, 3, 16, 512], BF16))
            wd = _st.enter_context(nc.sbuf_tensor(f"m{layer}_wd", [128, 3, 4, D], BF16))
            identb = _st.enter_context(nc.sbuf_tensor(f"m{layer}_id", [128, 128], BF16))
            xb = _st.enter_context(nc.sbuf_tensor(f"m{layer}_xb", [128, D], BF16))
            xT = _st.enter_context(nc.sbuf_tensor(f"m{layer}_xT", [128, 16, 128], BF16))
            sa = _st.enter_context(nc.sbuf_tensor(f"m{layer}_sa", [128, 512], F32))
            hm = _st.enter_context(nc.sbuf_tensor(f"m{layer}_hm", [128, 512], BF16))
            hmT = _st.enter_context(nc.sbuf_tensor(f"m{layer}_hmT", [128, 4, 128], BF16))
            yb = _st.enter_context(nc.sbuf_tensor(f"m{layer}_yb", [128, D], F32))
            pT = _st.enter_context(nc.psum_tensor(f"m{layer}_pT", [128, 16, 128], BF16))
            pA = _st.enter_context(nc.psum_tensor(f"m{layer}_pA", [128, 512], F32))
            pB = _st.enter_context(nc.psum_tensor(f"m{layer}_pB", [128, 512], F32))
            pY = _st.enter_context(nc.psum_tensor(f"m{layer}_pY", [128, 4, 512], F32))
            L = [nc.alloc_semaphore(name=f"ml{layer}_{i}") for i in range(14)]
            ENG = mybir.ALL_ENGINES
            rst = nc.alloc_registers(f"rst{layer}", engines=ENG)
            ren = nc.alloc_registers(f"ren{layer}", engines=ENG)
            iot = _st.enter_context(nc.sbuf_tensor(f"m{layer}_iot", [128, 1], F32))
            idxf = _st.enter_context(nc.sbuf_tensor(f"m{layer}_idxf", [128, 1], F32))
            idxi = _st.enter_context(nc.sbuf_tensor(f"m{layer}_idxi", [128, 1], I32))
            idsf = _st.enter_context(nc.sbuf_tensor(f"m{layer}_idsf", [128, 1], F32))
            idsi = _st.enter_context(nc.sbuf_tensor(f"m{layer}_idsi", [128, 1], I32))
            tr.dma("pool", lambda: pool.dma_start(out=identb[:], in_=ident_in), writes=["identb"])
            tr.dma("sp", lambda: sp.dma_start(out=iot[:], in_=iotap), writes=["iot"])

            def load_unit(u):
                ex, hf = u // 2, u % 2
                s = u % 3
                gsrc = w_gate[layer, ex].rearrange("(k p) n -> p k n", p=128)
                usrc = w_up[layer, ex].rearrange("(k p) n -> p k n", p=128)
                dsrc = w_down[layer, ex].rearrange("(k p) n -> p k n", p=128)
                for q in range(4):
                    tr.dma("pool", lambda: pool.dma_start(out=wg[:, s, q * 4:(q + 1) * 4, :], in_=gsrc[:, q * 4:(q + 1) * 4, hf * 512:(hf + 1) * 512]),
                           writes=[("w", s)])
                    tr.dma("pool", lambda: pool.dma_start(out=wu[:, s, q * 4:(q + 1) * 4, :], in_=usrc[:, q * 4:(q + 1) * 4, hf * 512:(hf + 1) * 512]),
                           writes=[("w", s)])
                    for hh_ in range(2):
                        tr.dma("pool", lambda: pool.dma_start(out=wd[:, s, q:q + 1, hh_ * 1024:(hh_ + 1) * 1024],
                                                              in_=dsrc[:, hf * 4 + q:hf * 4 + q + 1, hh_ * 1024:(hh_ + 1) * 1024]),
                               writes=[("w", s)])

            NU = 2 * NEX
            tr.flush()
            for sm_ in L:
                pool.sem_clear(sm_)
            rwi = pe.alloc_register(f"rwi{layer}")
            rwo = pool.alloc_register(f"rwo{layer}")
            pe.reg_mov(rwi, 16)
            pool.reg_mov(rwo, 0)
            nc.all_engine_barrier()
            load_unit(0)
            load_unit(1)
            for u in range(NU):
                ex, hf = u // 2, u % 2
                s = u % 3
                if u + 2 < NU:
                    load_unit(u + 2)
                tr.op("dve", lambda: dve.scalar_tensor_tensor(out=idxf[:], in0=R.stt[:, ex:ex + 1], scalar=128.0, in1=iot[:], op0=ALU.mult, op1=ALU.add),
                      reads=["stt", "iot"], writes=["idxf"])
                tr.op("dve", lambda: dve.tensor_copy(out=idxi[:], in_=idxf[:]), reads=["idxf"], writes=["idxi"])
                tr.op("dve", lambda: dve.tensor_scalar(out=idsf[:], in0=idxf[:], scalar1=-128.0, scalar2=None, op0=ALU.add),
                      reads=["idxf"], writes=["idsf"])
                tr.op("pe", lambda: pe.matmul(pA[:, 0:1], lhsT=wg[:, s, 0, 0:128], rhs=wd[:, s, 0, 0:1], start=True, stop=True),
                      reads=[("w", s), "identb"], writes=["pAdummy"])
                tr.wait("sp", (("c", "pe"), tr.cnt["pe"]))
                tr.wait("sp", (("c", "dve"), tr.cnt["dve"]))
                nc.all_engine_barrier()
                nc.regs_load(rst, R.sei[0:1, ex:ex + 1])
                nc.regs_load(ren, R.sei[0:1, NE + ex:NE + ex + 1])
                lid = nc.next_id()
                lstart, lend = f"moe_{lid}_loop", f"moe_{lid}_end"
                nc.br(lstart, engines=ENG)
                ydst = ys[hf]
                with nc.body(lstart, valid_engines=ENG):
                    mc = int(os.environ.get("MCUT", "9"))
                    if mc >= 1:
                        pool.indirect_dma_start(out=xb[:], out_offset=None, in_=xs[:, :],
                                                in_offset=bass.IndirectOffsetOnAxis(ap=idxi[:, 0:1], axis=0)).then_inc(L[0], 16)
                        pe.wait_ge(L[0], rwi)
                        pe.reg_add(rwi, rwi, 16)
                        pool.wait_ge(L[10], rwo)
                        pool.tensor_scalar(out=idsf[:], in0=idsf[:], scalar1=128.0, scalar2=None, op0=ALU.add).then_inc(L[11], 1)
                        pool.wait_ge(L[11], 1)
                        pool.tensor_copy(out=idsi[:], in_=idsf[:]).then_inc(L[12], 1)
                    if mc >= 2:
                        for k in range(16):
                            pe.transpose(pT[:, k, :], xb[:, k * 128:(k + 1) * 128], identb[:]).then_inc(L[1], 1)
                        pool.wait_ge(L[1], 1)
                        pool.tensor_scalar(out=idxf[:], in0=idxf[:], scalar1=128.0, scalar2=None, op0=ALU.add).then_inc(L[13], 1)
                        pool.wait_ge(L[13], 1)
                        pool.tensor_copy(out=idxi[:], in_=idxf[:]).then_inc(L[13], 1)
                        pool.wait_ge(L[13], 2)
                        dve.wait_ge(L[1], 8)
                        dve.tensor_copy(out=xT[:, 0:8, :], in_=pT[:, 0:8, :]).then_inc(L[2], 1)
                        act.wait_ge(L[1], 16)
                        act.copy(out=xT[:, 8:16, :], in_=pT[:, 8:16, :]).then_inc(L[2], 1)
                        pe.wait_ge(L[2], 2)
                    if mc >= 3:
                        for k in range(16):
                            mm = pe.matmul(pA[:], lhsT=xT[:, k, :], rhs=wg[:, s, k, :], start=(k == 0), stop=(k == 15))
                        mm.then_inc(L[3], 1)
                        for k in range(16):
                            mm = pe.matmul(pB[:], lhsT=xT[:, k, :], rhs=wu[:, s, k, :], start=(k == 0), stop=(k == 15))
                        mm.then_inc(L[3], 1)
                        act.wait_ge(L[3], 1)
                        act.activation(out=sa[:], in_=pA[:], func=AF.Silu).then_inc(L[4], 1)
                        dve.wait_ge(L[3], 2)
                        dve.wait_ge(L[4], 1)
                        dve.tensor_tensor(out=hm[:], in0=sa[:], in1=pB[:], op=ALU.mult).then_inc(L[5], 1)
                        pe.wait_ge(L[5], 1)
                    if mc >= 4:
                        for c in range(4):
                            pe.transpose(pT[:, c, :], hm[:, c * 128:(c + 1) * 128], identb[:]).then_inc(L[6], 1)
                        dve.wait_ge(L[6], 4)
                        dve.tensor_copy(out=hmT[:], in_=pT[:, 0:4, :]).then_inc(L[7], 1)
                        pe.wait_ge(L[7], 1)
                        for n in range(4):
                            for c in range(4):
                                mm = pe.matmul(pY[:, n, :], lhsT=hmT[:, c, :], rhs=wd[:, s, c, n * 512:(n + 1) * 512], start=(c == 0), stop=(c == 3))
                            mm.then_inc(L[8], 1)
                        act.wait_ge(L[8], 2)
                        act.wait_ge(L[12], 1)
                        act.copy(out=yb[:, 0:1024], in_=pY[:, 0:2, :]).then_inc(L[9], 1)
                        dve.wait_ge(L[8], 4)
                        dve.wait_ge(L[12], 1)
                        dve.tensor_copy(out=yb[:, 1024:2048], in_=pY[:, 2:4, :]).then_inc(L[9], 1)
                        pool.wait_ge(L[9], 2)
                    if mc >= 5:
                        pool.wait_ge(L[12], 1)
                        pool.indirect_dma_start(out=ydst[:, :], out_offset=bass.IndirectOffsetOnAxis(ap=idsi[:, 0:1], axis=0),
                                                in_=yb[:], in_offset=None).then_inc(L[10], 16)
                        pool.reg_add(rwo, rwo, 16)
                    nc.all_engine_barrier()
                    for si_, sm_ in enumerate(L):
                        if si_ in (0, 10):
                            continue
                        pool.sem_clear(sm_)
                    nc.all_engine_barrier()
                    nc.regs_alu(rst, rst, 1, op=ALU.add)
                    nc.br_lt(rst, ren, on_true=lstart, on_false=lend, engines=ENG)
                nc.switch_bb(lend)
                pool.wait_ge(L[10], rwo)
                nc.all_engine_barrier()
            tr.flush()

    def combine(R, layer, ntile, xsrc, final):
        with ExitStack() as _st:
            g2b = _st.enter_context(nc.sbuf_tensor(f"c{layer}_g2", [128, D], F32))
            lng2 = _st.enter_context(nc.sbuf_tensor(f"c{layer}_lng", [128, D], F32))
            lnb2 = _st.enter_context(nc.sbuf_tensor(f"c{layer}_lnb", [128, D], F32))
            yy = _st.enter_context(nc.sbuf_tensor(f"c{layer}_y", [128, 4, D], F32))
            xx = _st.enter_context(nc.sbuf_tensor(f"c{layer}_x", [128, D], F32))
            oo = _st.enter_context(nc.sbuf_tensor(f"c{layer}_o", [128, D], F32))
            mod_bcast(g2b, layer, 0, 5)
            tr.dma("sp", lambda: sp.dma_start(out=lng2[:], in_=ln_g[layer, 1:2, :].partition_broadcast(128)), writes=[lng2.name])
            tr.dma("sp", lambda: sp.dma_start(out=lnb2[:], in_=ln_b[layer, 1:2, :].partition_broadcast(128)), writes=[lnb2.name])
            for ti in range(ntile):
                for q, (ysrc, sl) in enumerate([(ys0, R.slAi), (ys1, R.slAi), (ys0, R.slBi), (ys1, R.slBi)]):
                    tr.dma("pool", lambda: pool.indirect_dma_start(out=yy[:, q, :], out_offset=None, in_=ysrc[:, :],
                                                                   in_offset=bass.IndirectOffsetOnAxis(ap=sl[:, ti:ti + 1], axis=0)),
                           reads=["ys"], writes=[("yy", q)])
                tr.dma("sp", lambda: sp.dma_start(out=xx[:], in_=xsrc[ti * 128:(ti + 1) * 128, :]), writes=["xx"])
                tr.op("pool", lambda: pool.tensor_tensor(out=yy[:, 0, :], in0=yy[:, 0, :], in1=yy[:, 1, :], op=ALU.add),
                      reads=[("yy", 0), ("yy", 1)], writes=[("yy", 0)])
                tr.op("pool", lambda: pool.tensor_tensor(out=yy[:, 2, :], in0=yy[:, 2, :], in1=yy[:, 3, :], op=ALU.add),
                      reads=[("yy", 2), ("yy", 3)], writes=[("yy", 2)])
                tr.op("dve", lambda: dve.tensor_scalar(out=yy[:, 0, :], in0=yy[:, 0, :], scalar1=R.gA[:, ti:ti + 1], scalar2=None, op0=ALU.mult),
                      reads=[("yy", 0)], writes=[("yy", 0)])
                tr.op("dve", lambda: dve.scalar_tensor_tensor(out=yy[:, 0, :], in0=yy[:, 2, :], scalar=R.gB[:, ti:ti + 1], in1=yy[:, 0, :],
                                                             op0=ALU.mult, op1=ALU.add),
                      reads=[("yy", 0), ("yy", 2)], writes=[("yy", 0)])
                tr.op("dve", lambda: dve.tensor_tensor(out=yy[:, 0, :], in0=yy[:, 0, :], in1=g2b[:], op=ALU.mult),
                      reads=[("yy", 0), g2b.name], writes=[("yy", 0)])
                tr.op("dve", lambda: dve.scalar_tensor_tensor(out=xx[:], in0=xx[:], scalar=ALPHA, in1=yy[:, 0, :], op0=ALU.mult, op1=ALU.add),
                      reads=["xx", ("yy", 0)], writes=["xx"])
                layer_norm(R, xx, "xx", lng2, lnb2, oo, "oo")
                if final:
                    tr.dma("sp", lambda: sp.dma_start(out=out[ti * 128:(ti + 1) * 128, :], in_=oo[:]), reads=["oo"], writes=["out"])
                else:
                    tr.dma("sp", lambda: sp.dma_start(out=x2s[ti * 128:(ti + 1) * 128, :], in_=oo[:]), reads=["oo"], writes=["x2s"])
            tr.flush()


    def layer0_post():
        with ExitStack() as stack:
            with ExitStack() as st2:
                R = alloc_router(stack, st2, 0, NT)
                e = st2.enter_context
                wo = e(nc.sbuf_tensor("p3_wo", [128, 16, D], BF16))
                g1b = e(nc.sbuf_tensor("p3_g1", [128, D], F32))
                oT = e(nc.sbuf_tensor("p3_oT", [128, 2, 16, 128], BF16))
                xt = e(nc.sbuf_tensor("p3_x", [128, 2, D], F32))
                zt = e(nc.sbuf_tensor("p3_z", [128, D], F32))
                pY = R.pTr[:].rearrange("p a b c -> p a (b c)")
                wsrc = w_o.rearrange("(k p) n -> p k n", p=128)
                for k4 in range(8):
                    for hh_ in range(2):
                        tr.dma("pool", lambda: pool.dma_start(out=wo[:, k4 * 2:(k4 + 1) * 2, hh_ * 1024:(hh_ + 1) * 1024],
                                                              in_=wsrc[:, k4 * 2:(k4 + 1) * 2, hh_ * 1024:(hh_ + 1) * 1024]), writes=["wo"])
                mod_bcast(g1b, 0, 0, 2)
                for ti in range(NT):
                    s = ti % 2
                    tr.dma("sp", lambda: sp.dma_start(out=oT[:, s], in_=OTs[:, :, ti * 128:(ti + 1) * 128].rearrange("h p t -> p h t")),
                           writes=[("oT", s)])
                    tr.dma("sp", lambda: sp.dma_start(out=xt[:, s, :], in_=xq[ti * 128:(ti + 1) * 128, :]), writes=[("xt3", s)])
                    for n in range(4):
                        for h in range(NH):
                            tr.op("pe", lambda: pe.matmul(pY[:, n, :], lhsT=oT[:, s, h, :], rhs=wo[:, h, n * 512:(n + 1) * 512],
                                                           start=(h == 0), stop=(h == NH - 1)),
                                  reads=[("oT", s), "wo"], writes=[("pTr", n)])
                    tr.op("dve", lambda: dve.tensor_tensor(out=zt[:], in0=R.pTr[:].rearrange("p a b c -> p (a b c)"), in1=g1b[:], op=ALU.mult),
                          reads=[("pTr", n) for n in range(4)] + [g1b.name], writes=["zt"])
                    tr.op("dve", lambda: dve.scalar_tensor_tensor(out=zt[:], in0=xt[:, s, :], scalar=ALPHA, in1=zt[:], op0=ALU.mult, op1=ALU.add),
                          reads=[("xt3", s), "zt"], writes=["zt"])
                    ln_router(R, ti, zt, "zt", x1s)
                tr.flush()
            with ExitStack() as st3:
                dispatch(R, st3)
                if stop_after >= 4:
                    moe_experts(0, R)
                if stop_after >= 5:
                    combine(R, 0, NT, x1s, final=False)

    def layer1():
        with ExitStack() as stack:
            with ExitStack() as st2:
                R = alloc_router(stack, st2, 1, 16)
                e = st2.enter_context
                wp = e(nc.sbuf_tensor("p6_wp", [128, 4, 4, 512], BF16))
                Bm = e(nc.sbuf_tensor("p6_B", [128, 4, 7, 128], BF16))
                g1b = e(nc.sbuf_tensor("p6_g1", [128, D], F32))
                uu = e(nc.sbuf_tensor("p6_u", [128, NT, D], BF16))
                xt = e(nc.sbuf_tensor("p6_x", [128, 2, D], F32))
                dT = e(nc.sbuf_tensor("p6_dT", [128, 16, 128], BF16))
                zt = e(nc.sbuf_tensor("p6_z", [128, D], F32))
                psb, s1p, s1h = zt, R.x1, R.hh
                pD_ = R.pTr[:].rearrange("p a b c -> p (a b) c")
                tr.dma("pool", lambda: pool.dma_start(out=wp[:], in_=w_pool.rearrange("g (k p) n -> p g k n", p=128)), writes=["wp"])
                tr.dma("pool", lambda: pool.dma_start(out=Bm[:].rearrange("p a b c -> p (a b c)"), in_=poolB), writes=["Bm"])
                tr.dma("sp", lambda: sp.dma_start(out=psb[:], in_=pool_scale.partition_broadcast(128)), writes=[psb.name])
                mod_bcast(g1b, 1, 0, 2)
                mod_bcast(s1p, 1, 0, 1)
                mod_bcast(s1h, 1, 0, 0)
                tr.op("pool", lambda: pool.tensor_scalar(out=s1p[:], in0=s1p[:], scalar1=1.0, scalar2=None, op0=ALU.add),
                      reads=[s1p.name], writes=[s1p.name])
                tr.op("pool", lambda: pool.tensor_tensor(out=g1b[:], in0=g1b[:], in1=psb[:], op=ALU.mult),
                      reads=[g1b.name, psb.name], writes=[g1b.name])
                for ti in range(NT):
                    s = ti % 2
                    tr.dma("sp", lambda: sp.dma_start(out=xt[:, s, :], in_=x2s[ti * 128:(ti + 1) * 128, :]), writes=[("xt6", s)])
                    tr.op("dve", lambda: dve.tensor_tensor(out=xt[:, s, :], in0=xt[:, s, :], in1=s1p[:], op=ALU.mult),
                          reads=[("xt6", s), s1p.name], writes=[("xt6", s)])
                    tr.op("pool", lambda: pool.tensor_tensor(out=uu[:, ti, :], in0=xt[:, s, :], in1=s1h[:], op=ALU.add),
                          reads=[("xt6", s), s1h.name], writes=[("uu", ti)])
                for ti in range(16):
                    s = ti % 2
                    srcs = [(ti - 1, 0) if ti > 0 else (16, 3), (ti, 5 if ti == 0 else (6 if ti == 15 else 1)),
                            (ti + 1, 2) if ti < 15 else (16, 4)]
                    for g in range(4):
                        for fc in range(4):
                            f = g * 4 + fc
                            for si, (st_, kind) in enumerate(srcs):
                                tr.op("pe", lambda: pe.matmul(pD_[:, f, :], lhsT=uu[:, st_, f * 128:(f + 1) * 128], rhs=Bm[:, g, kind, :],
                                                               start=(si == 0), stop=(si == 2)),
                                      reads=[("uu", st_), "Bm"], writes=[("pTr", f // 4)])
                    for g in range(4):
                        if g % 2 == 0:
                            tr.op("act", lambda: act.copy(out=dT[:, g * 4:(g + 1) * 4, :], in_=pD_[:, g * 4:(g + 1) * 4, :]),
                                  reads=[("pTr", g)], writes=[("dT", g)])
                        else:
                            tr.op("dve", lambda: dve.tensor_copy(out=dT[:, g * 4:(g + 1) * 4, :], in_=pD_[:, g * 4:(g + 1) * 4, :]),
                                  reads=[("pTr", g)], writes=[("dT", g)])
                    pY = R.pTr[:].rearrange("p a b c -> p a (b c)")
                    for g in range(4):
                        for fc in range(4):
                            tr.op("pe", lambda: pe.matmul(pY[:, g, :], lhsT=dT[:, g * 4 + fc, :], rhs=wp[:, g, fc, :], start=(fc == 0), stop=(fc == 3)),
                                  reads=[("dT", g), "wp"], writes=[("pTr", g)])
                    tr.dma("sp", lambda: sp.dma_start(out=xt[:, s, :], in_=x2s[ti * 128:(ti + 1) * 128, :]), writes=[("xt6", s)])
                    tr.op("dve", lambda: dve.tensor_tensor(out=zt[:], in0=R.pTr[:].rearrange("p a b c -> p (a b c)"), in1=g1b[:], op=ALU.mult),
                          reads=[("pTr", g) for g in range(4)] + [g1b.name], writes=["zt"])
                    tr.op("dve", lambda: dve.scalar_tensor_tensor(out=zt[:], in0=xt[:, s, :], scalar=ALPHA, in1=zt[:], op0=ALU.mult, op1=ALU.add),
                          reads=[("xt6", s), "zt"], writes=["zt"])
                    ln_router(R, ti, zt, "zt", x1s)
                tr.flush()
            with ExitStack() as st3:
                dispatch(R, st3)
                moe_experts(1, R)
                combine(R, 1, 16, x1s, final=True)

    if MOEONLY:
        sei_in = din("sei_in", [1, 2 * NE], I32)
        stt_in = din("stt_in", [1, NE], F32)
        with ExitStack() as _st:
            R = RouterState()
            R.sei = _st.enter_context(nc.sbuf_tensor("mo_sei", [128, 2 * NE], I32))
            R.stt = _st.enter_context(nc.sbuf_tensor("mo_stt", [128, NE], F32))
            tr.dma("sp", lambda: sp.dma_start(out=R.sei[:], in_=sei_in.partition_broadcast(128)), writes=["sei"])
            tr.dma("sp", lambda: sp.dma_start(out=R.stt[:], in_=stt_in.partition_broadcast(128)), writes=["stt"])
            tr.flush()
            moe_experts(0, R)
        tr.flush()
        return nc
    phase0()
    if stop_after >= 1:
        phase1()
    if stop_after >= 2:
        phase2()
    if stop_after >= 3:
        layer0_post()
    if stop_after >= 6:
        layer1()
    tr.flush()
    return nc


def _rope_tables(pos):
    pos = np.asarray(pos)
    row = (pos // 64).astype(np.float32)
    col = (pos % 64).astype(np.float32)
    inv = (np.float32(10000.0) ** (-np.arange(32, dtype=np.float32) / np.float32(32))).astype(np.float32)
    ar = row[None, :] * inv[:, None]
    ac = col[None, :] * inv[:, None]
    ang = np.concatenate([ar, ar, ac, ac], axis=0).astype(np.float32)
    cos = np.cos(ang).astype(np.float32)
    sin = np.sin(ang).astype(np.float32)
    sgn = np.ones((128, 1), np.float32)
    sgn[0:32] = -1
    sgn[64:96] = -1
    return cos, (sin * sgn).astype(np.float32)


def _pool_tables(qr):
    B = np.zeros((128, 4, 7, 128), np.float32)
    L = SEQ
    base = qr * OWN

    def fill(kind, g, out_tile, src_of):
        w = WINS[g]
        for oc in range(128):
            t = base + out_tile * 128 + oc
            lo = max(t - w // 2, 0)
            hi = min(t + w // 2, L)
            cntv = hi - lo
            for tp in range(lo, hi):
                r = src_of(tp)
                if r is not None:
                    B[r, g, kind, oc] += 1.0 / cntv
            r = src_of(t)
            if r is not None:
                B[r, g, kind, oc] -= 1.0

    for g in range(4):
        mid = 7
        fill(0, g, mid, lambda tp: (tp - (base + (mid - 1) * 128)) if base + (mid - 1) * 128 <= tp < base + mid * 128 else None)
        fill(1, g, mid, lambda tp: (tp - (base + mid * 128)) if base + mid * 128 <= tp < base + (mid + 1) * 128 else None)
        fill(2, g, mid, lambda tp: (tp - (base + (mid + 1) * 128)) if base + (mid + 1) * 128 <= tp < base + (mid + 2) * 128 else None)
        fill(3, g, 0, lambda tp: (tp - (base - 8)) if base - 8 <= tp < base else None)
        fill(4, g, 15, lambda tp: (8 + tp - (base + OWN)) if base + OWN <= tp < base + OWN + 8 else None)
        fill(5, g, 0, lambda tp: (tp - base) if base <= tp < base + 128 else None)
        fill(6, g, 15, lambda tp: (tp - (base + 15 * 128)) if base + 15 * 128 <= tp < base + 16 * 128 else None)
    return B.reshape(128, 4 * 7 * 128)


def make_in_maps(inputs, cores=range(8)):
    x = np.asarray(inputs["x"], np.float32)
    c = np.asarray(inputs["c"], np.float32)
    ctx = np.asarray(inputs["ctx"], np.float32)
    c_ctx = np.asarray(inputs["c_ctx"], np.float32)
    ident = np.eye(128, dtype=np.float32)
    perm = np.zeros((128, 128), np.float32)
    for m in range(128):
        perm[m + 32 if (m % 64) < 32 else m - 32, m] = 1.0
    triu = np.triu(np.ones((128, 128), np.float32), k=1)
    cosK, sinK = _rope_tables(np.arange(SEQ))
    shared = {
        "w_mod": np.asarray(inputs["w_mod"], np.float32), "b_mod": np.asarray(inputs["b_mod"], np.float32),
        "ln_g": np.asarray(inputs["ln_g"], np.float32), "ln_b": np.asarray(inputs["ln_b"], np.float32),
        "w_qkv": np.asarray(inputs["w_qkv"], np.float32)[0],
        "q_gain": np.asarray(inputs["q_gain"], np.float32).reshape(128, 1),
        "k_gain": np.asarray(inputs["k_gain"], np.float32).reshape(128, 1),
        "w_o": np.asarray(inputs["w_o"], np.float32)[0],
        "w_pool": np.asarray(inputs["w_pool"], np.float32)[0],
        "pool_scale": np.asarray(inputs["pool_scale"], np.float32).reshape(1, D),
        "w_router": np.asarray(inputs["w_router"], np.float32),
        "router_bias": np.asarray(inputs["router_bias"], np.float32).reshape(1, NE),
        "w_gate": np.asarray(inputs["w_gate"], np.float32), "w_up": np.asarray(inputs["w_up"], np.float32),
        "w_down": np.asarray(inputs["w_down"], np.float32),
        "iotap": np.arange(128, dtype=np.float32).reshape(128, 1), "ident": ident, "perm": perm, "triu": triu, "cosK": cosK, "sinK": sinK,
    }
    maps = []
    for core in cores:
        b, qr = core // 4, core % 4
        base = qr * OWN
        loc = list(range(base, base + OWN))
        halo = [min(max(t, 0), SEQ - 1) for t in list(range(base - 8, base)) + list(range(base + OWN, base + OWN + 8))]
        loc = loc + halo + [base] * (NTOK - OWN - 16)
        loc = np.asarray(loc)
        cpad = np.zeros((128, 16, 33), np.float32)
        cpad[:, :, 0] = c[b].reshape(16, 128).T
        cpad[:, :, 32] = c_ctx.reshape(16, 128).T
        cosQ, sinQ = _rope_tables(loc)
        m = dict(shared)
        m.update({
            "keysrc": np.concatenate([ctx[b], x[b]], axis=0),
            "xq": x[b][loc],
            "cpad": cpad.reshape(128, 16 * 33),
            "cosQ": cosQ, "sinQ": sinQ,
            "poolB": _pool_tables(qr),
        })
        maps.append(m)
    return maps


def kernel(**inputs):
    nc = build()
    maps = make_in_maps(inputs)
    res = run_bass_kernel_spmd(nc, maps, core_ids=list(range(8)))
    outs = [np.asarray(r["out"], np.float32) for r in res.results]
    full = np.zeros((2, SEQ, D), np.float32)
    for core in range(8):
        b, qr = core // 4, core % 4
        full[b, qr * OWN:(qr + 1) * OWN] = outs[core]
    return full
```
